# Optimizing a Trainium2 kernel written in Bass

```python
import jax, jax.numpy as jnp
from jax import lax
import numpy as np

D_MODEL = 2048
BATCH = 2
SEQ = 8192
DEPTH = 1
DEC_BATCH = 1
DEC_SEQ = 8192
PAST_LEN = 128

CONV_WIDTH = D_MODEL // 2
CONV_GROUPS = 8
CONV_K = 3
SGU_WIDTH = D_MODEL // 2
SGU_HEADS = 8
SGU_HEAD_DIM = SGU_WIDTH // SGU_HEADS
CHUNK = 128
MIX_WIDTH = CONV_WIDTH + SGU_WIDTH
PROJ_WIDTH = 3 * CONV_WIDTH + 2 * SGU_WIDTH
N_EXPERTS = 64
TOP_K = 8
N_EXPERT_GROUPS = 8
TOPK_GROUPS = 4
EXPERT_FF = 512
SHARED_FF = 512
ROUTED_SCALE = 2.5
MOE_BLOCK = 256
EPS = 1e-6

kernel_name = "hybrid_conv_sgu_moe_encoder"


def rms_norm(x, g):
    xf = x.astype(jnp.float32)
    y = xf * lax.rsqrt(jnp.mean(xf * xf, axis=-1, keepdims=True) + EPS)
    return (y * g.astype(jnp.float32)).astype(x.dtype)


def modulate(h, shift, scale):
    return h * (1 + scale[:, None, :]) + shift[:, None, :]


def swiglu(x, wg, wu, wd):
    return (jax.nn.silu(x @ wg) * (x @ wu)) @ wd


def short_conv_mixer(b_gate, c_gate, xh, conv_w):
    z = c_gate * xh
    zp = jnp.pad(z, ((0, 0), (1, 1), (0, 0)))
    conv = zp[:, :-2] * conv_w[0] + zp[:, 1:-1] * conv_w[1] + zp[:, 2:] * conv_w[2]
    return b_gate * conv


def spatial_gating_mixer(u, v, ln_g, ln_b, w_s, b_s):
    bn, s, _ = v.shape
    u = jax.nn.gelu(u)
    v = jax.nn.gelu(v)
    vh = v.reshape(bn, s // CHUNK, CHUNK, SGU_HEADS, SGU_HEAD_DIM)
    vf = vh.astype(jnp.float32)
    mu = jnp.mean(vf, axis=-1, keepdims=True)
    var = jnp.mean(jnp.square(vf - mu), axis=-1, keepdims=True)
    vn = ((vf - mu) * lax.rsqrt(var + EPS)).astype(v.dtype)
    vn = vn * ln_g.reshape(SGU_HEADS, SGU_HEAD_DIM) + ln_b.reshape(SGU_HEADS, SGU_HEAD_DIM)
    sp = jnp.einsum('hpq,bcqhd->bcphd', w_s, vn) + b_s.T[None, None, :, :, None]
    return u * sp.reshape(bn, s, SGU_WIDTH)


def route(h, w_r, e_bias):
    t = h.shape[0]
    scores = jax.nn.sigmoid(jnp.einsum('td,de->te', h.astype(jnp.float32), w_r.astype(jnp.float32)))
    sel = scores + e_bias.astype(jnp.float32)[None, :]
    grp = sel.reshape(t, N_EXPERT_GROUPS, N_EXPERTS // N_EXPERT_GROUPS)
    grp_score = jnp.sum(lax.top_k(grp, 2)[0], axis=-1)
    _, top_g = lax.top_k(grp_score, TOPK_GROUPS)
    gmask = jnp.any(top_g[..., None] == jnp.arange(N_EXPERT_GROUPS)[None, None, :], axis=1)
    emask = jnp.repeat(gmask, N_EXPERTS // N_EXPERT_GROUPS, axis=-1)
    masked = jnp.where(emask, sel, -jnp.inf)
    _, idx = lax.top_k(masked, TOP_K)
    w = jnp.take_along_axis(scores, idx, axis=-1)
    w = w / (jnp.sum(w, axis=-1, keepdims=True) + 1e-20) * ROUTED_SCALE
    return idx, w


def routed_experts(x, idx, gates, wg, wu, wd):
    t, d = x.shape
    tk = t * TOP_K
    flat_e = idx.reshape(tk)
    order = jnp.argsort(flat_e)
    e_sorted = flat_e[order]
    tok_sorted = order // TOP_K
    gate_sorted = gates.reshape(tk)[order]
    counts = jnp.bincount(flat_e, length=N_EXPERTS)
    padded = (counts + MOE_BLOCK - 1) // MOE_BLOCK * MOE_BLOCK
    start = jnp.cumsum(counts) - counts
    pad_end = jnp.cumsum(padded)
    pad_start = pad_end - padded
    dest = pad_start[e_sorted] + jnp.arange(tk) - start[e_sorted]
    n_blocks = -(-tk // MOE_BLOCK) + N_EXPERTS
    slot_tok = jnp.full((n_blocks * MOE_BLOCK,), t, dtype=tok_sorted.dtype).at[dest].set(tok_sorted)
    block_e = jnp.minimum(jnp.searchsorted(pad_end, jnp.arange(n_blocks) * MOE_BLOCK, side='right'),
                          N_EXPERTS - 1)
    x_pad = jnp.concatenate([x, jnp.zeros((1, d), x.dtype)], axis=0)
    xb = x_pad[slot_tok].reshape(n_blocks, MOE_BLOCK, d)

    def expert_block(args):
        xblk, e = args
        return swiglu(xblk, wg[e], wu[e], wd[e])

    yb = lax.map(expert_block, (xb, block_e)).reshape(n_blocks * MOE_BLOCK, d)
    y_sorted = yb[dest] * gate_sorted[:, None].astype(x.dtype)
    return jnp.zeros((t, d), x.dtype).at[tok_sorted].add(y_sorted)


def encoder_layer(x, c, w_ada, b_ada, g_pre_mix, w_in, conv_w, ln_v_g, ln_v_b, w_spatial, b_spatial,
                  g_out_a, g_out_b, w_out, g_post_mix, g_pre_ffn, w_router, router_bias,
                  w_exp_gate, w_exp_up, w_exp_down, w_sh_gate, w_sh_up, w_sh_down, g_post_ffn):
    bn, s, d = x.shape
    mod = jax.nn.silu(c) @ w_ada + b_ada
    sh1, sc1, gt1, sh2, sc2, gt2 = jnp.split(mod, 6, axis=-1)
    h = modulate(rms_norm(x, g_pre_mix), sh1, sc1)
    proj = h @ w_in
    b_g, c_g, xh, u, v = jnp.split(
        proj, [CONV_WIDTH, 2 * CONV_WIDTH, 3 * CONV_WIDTH, 3 * CONV_WIDTH + SGU_WIDTH], axis=-1)
    ya = rms_norm(short_conv_mixer(b_g, c_g, xh, conv_w), g_out_a)
    yb = rms_norm(spatial_gating_mixer(u, v, ln_v_g, ln_v_b, w_spatial, b_spatial), g_out_b)
    mix = jnp.concatenate([ya, yb], axis=-1) @ w_out
    x = x + gt1[:, None, :] * rms_norm(mix, g_post_mix)
    h = modulate(rms_norm(x, g_pre_ffn), sh2, sc2).reshape(bn * s, d)
    idx, gates = route(h, w_router, router_bias)
    ffn = routed_experts(h, idx, gates, w_exp_gate, w_exp_up, w_exp_down) + swiglu(h, w_sh_gate, w_sh_up, w_sh_down)
    x = x + gt2[:, None, :] * rms_norm(ffn.reshape(bn, s, d), g_post_ffn)
    return x


def setup_inputs(seed: int = 0) -> dict:
    key = jax.random.key(seed)
    ks = jax.random.split(key, 32)
    f32 = jnp.float32
    L, D = DEPTH, D_MODEL

    def nrm(k, shape, scale):
        return jax.random.normal(k, shape, f32) * scale

    def gain(k, shape):
        return 1.0 + 0.01 * jax.random.normal(k, shape, f32)

    return {
        "x_prompt": nrm(ks[0], (BATCH, SEQ, D), 1.0),
        "x_sample": nrm(ks[1], (DEC_BATCH, DEC_SEQ, D), 1.0),
        "c_prompt": nrm(ks[2], (BATCH, D), 1.0),
        "c_sample": nrm(ks[3], (DEC_BATCH, D), 1.0),
        "w_ada": nrm(ks[4], (L, D, 6 * D), 0.5 * D ** -0.5),
        "b_ada": nrm(ks[5], (L, 6 * D), 0.01),
        "g_pre_mix": gain(ks[6], (L, D)),
        "w_in": nrm(ks[7], (L, D, PROJ_WIDTH), D ** -0.5),
        "conv_w": nrm(ks[8], (L, CONV_K, CONV_WIDTH), CONV_K ** -0.5),
        "ln_v_g": gain(ks[9], (L, SGU_WIDTH)),
        "ln_v_b": nrm(ks[10], (L, SGU_WIDTH), 0.01),
        "w_spatial": nrm(ks[11], (L, SGU_HEADS, CHUNK, CHUNK), 0.5 * CHUNK ** -0.5),
        "b_spatial": gain(ks[12], (L, SGU_HEADS, CHUNK)),
        "g_out_a": gain(ks[13], (L, CONV_WIDTH)),
        "g_out_b": gain(ks[14], (L, SGU_WIDTH)),
        "w_out": nrm(ks[15], (L, MIX_WIDTH, D), MIX_WIDTH ** -0.5),
        "g_post_mix": gain(ks[16], (L, D)),
        "g_pre_ffn": gain(ks[17], (L, D)),
        "w_router": nrm(ks[18], (L, D, N_EXPERTS), D ** -0.5),
        "router_bias": nrm(ks[19], (L, N_EXPERTS), 0.01),
        "w_exp_gate": nrm(ks[20], (L, N_EXPERTS, D, EXPERT_FF), D ** -0.5),
        "w_exp_up": nrm(ks[21], (L, N_EXPERTS, D, EXPERT_FF), D ** -0.5),
        "w_exp_down": nrm(ks[22], (L, N_EXPERTS, EXPERT_FF, D), EXPERT_FF ** -0.5),
        "w_sh_gate": nrm(ks[23], (L, D, SHARED_FF), D ** -0.5),
        "w_sh_up": nrm(ks[24], (L, D, SHARED_FF), D ** -0.5),
        "w_sh_down": nrm(ks[25], (L, SHARED_FF, D), SHARED_FF ** -0.5),
        "g_post_ffn": gain(ks[26], (L, D)),
    }


def reference(x_prompt, x_sample, c_prompt, c_sample, w_ada, b_ada, g_pre_mix, w_in, conv_w, ln_v_g,
              ln_v_b, w_spatial, b_spatial, g_out_a, g_out_b, w_out, g_post_mix, g_pre_ffn, w_router,
              router_bias, w_exp_gate, w_exp_up, w_exp_down, w_sh_gate, w_sh_up, w_sh_down, g_post_ffn):
    y_prompt = x_prompt
    y_sample = x_sample
    for l in range(DEPTH):
        layer_w = (w_ada[l], b_ada[l], g_pre_mix[l], w_in[l], conv_w[l], ln_v_g[l], ln_v_b[l],
                   w_spatial[l], b_spatial[l], g_out_a[l], g_out_b[l], w_out[l], g_post_mix[l],
                   g_pre_ffn[l], w_router[l], router_bias[l], w_exp_gate[l], w_exp_up[l], w_exp_down[l],
                   w_sh_gate[l], w_sh_up[l], w_sh_down[l], g_post_ffn[l])
        y_prompt = encoder_layer(y_prompt, c_prompt, *layer_w)
        y_sample = encoder_layer(y_sample, c_sample, *layer_w)
    return (y_prompt, y_sample)
```

```python
from contextlib import ExitStack
import numpy as np
import concourse.bass as bass
import concourse.mybir as mybir
from concourse.bass_utils import run_bass_kernel_spmd

F32 = mybir.dt.float32
BF16 = mybir.dt.bfloat16
I32 = mybir.dt.int32
U32 = mybir.dt.uint32
AF = mybir.ActivationFunctionType
ALU = mybir.AluOpType
AX = mybir.AxisListType

NCORES = 8
D = 2048
NSEG = 3
SEGT = 1024
NTOK = NSEG * SEGT
NT = NTOK // 128
GRP = 512
NG = NTOK // GRP
NE = 64
FF = 512
DYN_SKIP = True
JBLK = 8
CAP = JBLK * 128
EPS = 1e-6
BIG = 1.0e4


class _Dummy:
    def __getattr__(self, k):
        return lambda *a, **kw: _Dummy()

    def __getitem__(self, k):
        return _Dummy()


class Sync:
    def __init__(self, nc, stack):
        self.nc = nc
        self.stack = stack
        self.ev = {}
        self.sems = {}
        self.counts = {}
        self.dry = True

    def sem(self, name):
        if name not in self.sems:
            self.sems[name] = self.stack.enter_context(self.nc.semaphore(name))
            self.counts[name] = 0
        return self.sems[name]

    def reset_pass(self, dry):
        self.dry = dry
        for k in self.counts:
            self.counts[k] = 0


class Stream:
    def __init__(self, sy, name):
        self.sy = sy
        self.name = name
        self.semname = "s_" + name
        sy.sem(self.semname)
        self.eng = _Dummy()
        self.waited = {}

    def begin(self, eng):
        self.eng = _Dummy() if self.sy.dry else eng
        self.waited = {}

    @property
    def e(self):
        return self.eng

    def _rec(self, ev, sn, n):
        sy = self.sy
        if ev is None:
            return
        if sy.dry:
            assert ev not in sy.ev, ev
            sy.ev[ev] = (sn, n)
        else:
            assert sy.ev[ev] == (sn, n), (ev, sy.ev[ev], sn, n)

    def op(self, instr, ev=None):
        sy = self.sy
        sy.counts[self.semname] += 1
        n = sy.counts[self.semname]
        if not sy.dry:
            instr.then_inc(sy.sems[self.semname], 1)
        self._rec(ev, self.semname, n)
        return n

    def dep(self):
        if not self.sy.dry:
            n = self.sy.counts[self.semname]
            if n > 0 and self.waited.get(self.semname, 0) < n:
                self.eng.wait_ge(self.sy.sems[self.semname], n)
                self.waited[self.semname] = n

    def dma(self, key, out, in_, ev=None, **kw):
        sy = self.sy
        sn = "d_" + key
        sy.sem(sn)
        sy.counts[sn] += 16
        n = sy.counts[sn]
        if not sy.dry:
            self.eng.dma_start(out=out, in_=in_, **kw).then_inc(sy.sems[sn], 16)
        self._rec(ev, sn, n)
        return n

    def idma(self, key, ev=None, **kw):
        sy = self.sy
        sn = "d_" + key
        sy.sem(sn)
        sy.counts[sn] += 16
        n = sy.counts[sn]
        if not sy.dry:
            oi = kw.pop("out_idx", None)
            ii = kw.pop("in_idx", None)
            kw["out_offset"] = bass.IndirectOffsetOnAxis(ap=oi, axis=0) if oi is not None else None
            kw["in_offset"] = bass.IndirectOffsetOnAxis(ap=ii, axis=0) if ii is not None else None
            self.eng.indirect_dma_start(**kw).then_inc(sy.sems[sn], 16)
        self._rec(ev, sn, n)
        return n

    def piece(self, cond, body, skip):
        sy = self.sy
        if sy.dry or cond is None:
            body()
            return
        n0 = sy.counts[self.semname]
        w0 = dict(self.waited)
        with self.eng.If_cmp(cond[0], cond[1], "IS_GT"):
            body()
        k = sy.counts[self.semname] - n0
        w1 = dict(self.waited)
        self.waited = w0
        with self.eng.Else():
            skip(k)
        self.waited = {kk: min(w1.get(kk, 0), self.waited.get(kk, 0)) for kk in set(w1) | set(self.waited)}

    def emit_expert(self, pieces, reg, tiny, always=2):
        sy = self.sy
        if sy.dry or reg is None:
            for (j, k, body) in pieces:
                body()
            return
        sn = self.semname

        def rec(i, lo):
            while i < len(pieces) and pieces[i][0] <= lo:
                pieces[i][2]()
                i += 1
            if i == len(pieces):
                return
            n0 = sy.counts[sn]
            w0 = dict(self.waited)
            n_end = n0 + sum(k for (j, k, body) in pieces[i:])
            with self.eng.If_cmp(reg, (lo + 1) * 128, "IS_LE"):
                pend = 0
                for (j, k, body) in pieces[i:]:
                    if j <= lo:
                        if pend:
                            tiny(pend)
                            sy.counts[sn] += pend
                            pend = 0
                        body()
                    else:
                        pend += k
                if pend:
                    tiny(pend)
                    sy.counts[sn] += pend
            assert sy.counts[sn] == n_end, (sy.counts[sn], n_end)
            w_a = dict(self.waited)
            sy.counts[sn] = n0
            self.waited = w0
            with self.eng.Else():
                pieces[i][2]()
                rec(i + 1, lo + 1)
            assert sy.counts[sn] == n_end, (sy.counts[sn], n_end)
            self.waited = {kk: min(w_a.get(kk, 0), self.waited.get(kk, 0)) for kk in set(w_a) | set(self.waited)}
        rec(0, always - 1)

    def tick(self, instr, k):
        if not self.sy.dry and k > 0:
            instr.then_inc(self.sy.sems[self.semname], k)

    def wait(self, *evs):
        if self.sy.dry:
            return
        for ev in evs:
            if ev is None:
                continue
            sn, n = self.sy.ev[ev]
            if self.waited.get(sn, 0) >= n:
                continue
            self.eng.wait_ge(self.sy.sems[sn], n)
            self.waited[sn] = n


class Ctx:
    def __init__(self, nc, sy, dram):
        self.nc = nc
        self.sy = sy
        self.dr = dram
        self.S = {n: Stream(sy, n) for n in ("pe", "act", "dve", "pool", "sp")}
        self.P = {}

    def alloc(self, st, specs):
        T = {}
        for name, shape, dt, *kind in specs:
            if self.sy.dry:
                T[name] = _Dummy()
            elif kind and kind[0] == "psum":
                T[name] = st.enter_context(self.nc.psum_tensor(f"t{int(self.sy.dry)}_" + name, list(shape), dt))
            else:
                T[name] = st.enter_context(self.nc.sbuf_tensor(f"t{int(self.sy.dry)}_" + name, list(shape), dt))
        return T

    def phase(self, name, specs, progs):
        S = self.S
        self.uid = getattr(self, "uid", 0) + 1
        name = f"{name}u{self.uid}"
        with ExitStack() as st:
            T = self.alloc(st, [(name + "_" + s[0],) + tuple(s[1:]) for s in specs])
            T = {k[len(name) + 1:]: v for k, v in T.items()}
            T.update(self.P)
            if self.sy.dry:
                for en in ("sp", "pool", "act", "dve", "pe"):
                    if en in progs:
                        S[en].begin(None)
                        progs[en](T)
            else:
                with self.nc.Block() as blk:
                    reg = {"sp": blk.sync, "pool": blk.gpsimd, "act": blk.scalar, "dve": blk.vector, "pe": blk.tensor}
                    for en in ("sp", "pool", "act", "dve", "pe"):
                        if en in progs:
                            def f(eng, en=en):
                                S[en].begin(eng)
                                progs[en](T)
                            reg[en](f)


def bcast3(ap2d, a, b):
    return ap2d.unsqueeze(2).to_broadcast([128, a, b])


def phase0(cx):
    S = cx.S
    dr = cx.dr
    SP, PL, ACT, DVE, PE = S["sp"], S["pool"], S["act"], S["dve"], S["pe"]
    NB = 48
    specs = [
        ("cT", [128, 3, 16], F32), ("wa0", [128, 16, 256], F32), ("wa1", [128, 16, 256], F32), ("wa2", [128, 16, 256], F32),
        ("mod", [3, 6 * D], F32), ("der", [3, 4, D], F32),
        ("wsf", [128, 8, 128], F32),
        ("pm0", [128, 512], F32, "psum"), ("pm1", [128, 512], F32, "psum"),
        ("pws", [128, 8, 128], F32, "psum"),
    ]

    def sp(T):
        for b in range(3):
            SP.dma("c", T["cT"][:, b, :], dr["cvec"][b:b + 1, :].rearrange("o (k p) -> p (o k)", p=128), allow_slow_non_contiguous=True)
        SP.dma("c", T["mod"][:], dr["b_ada"].partition_broadcast(3))
        for i, nm in enumerate(("g_pre_mix", "g_post_mix", "g_pre_ffn", "g_post_ffn")):
            SP.dma("c", T["der"][:, i, :], dr[nm].partition_broadcast(3), ev=("c_all" if i == 3 else None))
        for b in range(NB):
            if b >= 3:
                SP.wait(f"mod_pe{b - 3}")
            SP.dma(f"wa{b % 3}", T[f"wa{b % 3}"][:], dr["w_ada"][:, b * 256:(b + 1) * 256].rearrange("(k p) n -> p k n", p=128), ev=f"wa_ld{b}")
        SP.dma("k", T["identf"][:], dr["ident"])
        SP.dma("k", T["wsf"][:], dr["w_spatial"].rearrange("h p q -> p h q"))
        SP.dma("k", T["bsb"][:], dr["b_spatial"].partition_broadcast(128))
        SP.dma("k", T["lngb"][:], dr["ln_v_g"].partition_broadcast(128))
        SP.dma("k", T["lnbb"][:], dr["ln_v_b"].partition_broadcast(128))
        for t in range(3):
            SP.dma("k", T["convw"][:, t, :], dr["conv_w"][t:t + 1, :].rearrange("o (c p) -> p (o c)", p=128), allow_slow_non_contiguous=True)
        SP.dma("k", T["goa"][:], dr["g_out_a"].rearrange("o (c p) -> p (o c)", p=128), allow_slow_non_contiguous=True)
        SP.dma("k", T["gob"][:], dr["g_out_b"].rearrange("o (c p) -> p (o c)", p=128), allow_slow_non_contiguous=True)
        SP.dma("k", T["wr"][:], dr["w_router"].rearrange("(k p) e -> p k e", p=128))
        SP.dma("k", T["rbias"][:], dr["router_bias"].partition_broadcast(128))
        SP.dma("k", T["ecap"][:], dr["ecap"].partition_broadcast(128))
        SP.dma("k", T["hmask"][:], dr["hmask"], ev="k_all")
        SP.wait("der_done")
        for i in range(4):
            SP.dma("mv", dr["modv"][i], T["der"][:, i, :])
        SP.dma("mv", dr["modv"][4], T["mod"][:, 0:D])
        SP.dma("mv", dr["modv"][5], T["mod"][:, 3 * D:4 * D], ev="modv_st")
        SP.wait("modv_st")

    def pool(T):
        PL.dma("kb", T["identb"][:], dr["ident"])
        PL.dma("kb", T["trib"][:], dr["tri"], ev="kb_all")
        PL.op(PL.e.memset(T["zerob"][:], 0.0))
        PL.op(PL.e.memset(T["onesb"][:], 1.0), ev="ones_set")
        PL.wait("kb_all")

    def act(T):
        ACT.wait("c_all")
        ACT.op(ACT.e.activation(out=T["cT"][:], in_=T["cT"][:], func=AF.Silu), ev="silu_c")
        ACT.wait("wsT_pe")
        ACT.op(ACT.e.copy(out=T["wsT"][:], in_=T["pws"][:]), ev="wsT_done")

    def dve(T):
        DVE.wait("c_all")
        mod = T["mod"]
        for b in range(NB):
            DVE.wait(f"mod_pe{b}")
            pm = T[f"pm{b % 2}"]
            DVE.op(DVE.e.tensor_tensor(out=mod[:, b * 256:(b + 1) * 256], in0=pm[0:3, 0:256], in1=mod[:, b * 256:(b + 1) * 256], op=ALU.add), ev=f"mod_ev{b}")
        DVE.dep()
        DVE.op(DVE.e.scalar_tensor_tensor(out=T["der"][:, 0, :], in0=mod[:, D:2 * D], scalar=1.0, in1=T["der"][:, 0, :], op0=ALU.add, op1=ALU.mult))
        DVE.op(DVE.e.tensor_tensor(out=T["der"][:, 1, :], in0=mod[:, 2 * D:3 * D], in1=T["der"][:, 1, :], op=ALU.mult))
        DVE.op(DVE.e.scalar_tensor_tensor(out=T["der"][:, 2, :], in0=mod[:, 4 * D:5 * D], scalar=1.0, in1=T["der"][:, 2, :], op0=ALU.add, op1=ALU.mult))
        DVE.op(DVE.e.tensor_tensor(out=T["der"][:, 3, :], in0=mod[:, 5 * D:6 * D], in1=T["der"][:, 3, :], op=ALU.mult), ev="der_done")

    def pe(T):
        PE.wait("silu_c")
        for b in range(NB):
            PE.wait(f"wa_ld{b}")
            if b >= 2:
                PE.wait(f"mod_ev{b - 2}")
            pm = T[f"pm{b % 2}"]
            wa = T[f"wa{b % 3}"]
            for k in range(16):
                mm = PE.e.matmul(pm[0:3, 0:256], T["cT"][:, :, k], wa[:, k, :], start=(k == 0), stop=(k == 15))
            PE.op(mm, ev=f"mod_pe{b}")
        PE.wait("k_all")
        for h in range(8):
            mm = PE.e.transpose(out=T["pws"][:, h, :], in_=T["wsf"][:, h, :], identity=T["identf"][:])
        PE.op(mm, ev="wsT_pe")

    cx.phase("p0", specs, {"sp": sp, "pool": pool, "act": act, "dve": dve, "pe": pe})


def phase0b(cx):
    S = cx.S
    dr = cx.dr
    SP, PL, ACT, DVE, PE = S["sp"], S["pool"], S["act"], S["dve"], S["pe"]
    specs = [
        ("xh", [12, D], F32), ("a1h", [12, D], F32), ("b1h", [12, D], F32), ("hjunk", [12, D], BF16),
        ("htmp", [12, D], F32), ("hh", [12, D], BF16), ("hss", [12, 2], F32),
        ("pth", [128, 16, 12], BF16, "psum"),
    ]

    def sp(T):
        SP.dma("hl", T["xh"][:], dr["xhalo"])
        for s in range(NSEG):
            SP.dma("hl", T["a1h"][4 * s:4 * s + 4, :], dr["modv"][0, s:s + 1, :].partition_broadcast(4))
            SP.dma("hl", T["b1h"][4 * s:4 * s + 4, :], dr["modv"][4, s:s + 1, :].partition_broadcast(4), ev=("hl_all" if s == NSEG - 1 else None))

    def act(T):
        ACT.wait("hl_all")
        ACT.op(ACT.e.activation(out=T["hjunk"][:], in_=T["xh"][:], func=AF.Square, accum_out=T["hss"][:, 0:1]), ev="h_ssq")
        ACT.wait("h_var")
        ACT.op(ACT.e.activation(out=T["hss"][:, 1:2], in_=T["hss"][:, 1:2], func=AF.Sqrt), ev="h_sqrt")

    def dve(T):
        DVE.wait("h_ssq")
        DVE.op(DVE.e.tensor_scalar(out=T["hss"][:, 1:2], in0=T["hss"][:, 0:1], scalar1=1.0 / D, scalar2=EPS, op0=ALU.mult, op1=ALU.add), ev="h_var")
        DVE.wait("h_sqrt")
        DVE.op(DVE.e.reciprocal(out=T["hss"][:, 1:2], in_=T["hss"][:, 1:2]))
        DVE.dep()
        DVE.op(DVE.e.scalar_tensor_tensor(out=T["htmp"][:], in0=T["xh"][:], scalar=T["hss"][:, 1:2], in1=T["a1h"][:], op0=ALU.mult, op1=ALU.mult))
        DVE.dep()
        DVE.op(DVE.e.tensor_tensor(out=T["hh"][:], in0=T["htmp"][:], in1=T["b1h"][:], op=ALU.add), ev="hh_done")
        DVE.wait("hT_pe")
        DVE.op(DVE.e.tensor_copy(out=T["hTh"][:], in_=T["pth"][:]), ev="hTh_done")

    def pe(T):
        PE.wait("hh_done")
        for k in range(16):
            mm = PE.e.transpose(out=T["pth"][:, k, :], in_=T["hh"][:, k * 128:(k + 1) * 128], identity=T["identb"][0:12, 0:12])
        PE.op(mm, ev="hT_pe")

    cx.phase("p0b", specs, {"sp": sp, "act": act, "dve": dve, "pe": pe})


def phaseP1(cx, g):
    S = cx.S
    dr = cx.dr
    SP, PL, ACT, DVE, PE = S["sp"], S["pool"], S["act"], S["dve"], S["pe"]
    s = g // 2
    specs = [(f"xt{i}", [128, D], F32) for i in range(4)] + [
        ("junk", [128, D], BF16), ("tmpf", [128, D], F32), ("htok0", [128, D], BF16), ("htok1", [128, D], BF16),
        ("A1b", [128, D], F32), ("B1b", [128, D], F32), ("ss", [128, 8], F32),
        ("pt0", [128, 16, 128], BF16, "psum"), ("pt1", [128, 16, 128], BF16, "psum"),
    ]
    G = f"{g}"

    def sp(T):
        SP.dma("ab", T["A1b"][:], dr["modv"][0, s:s + 1, :].partition_broadcast(128))
        SP.dma("ab", T["B1b"][:], dr["modv"][4, s:s + 1, :].partition_broadcast(128), ev="ab_ld" + G)
        for i in range(4):
            r0 = g * GRP + i * 128
            SP.dma(f"xt{i}", T[f"xt{i}"][:], dr["xs"][r0:r0 + 128, :], ev=f"xt_ld{G}_{i}")

    def act(T):
        for i in range(4):
            ACT.wait(f"xt_ld{G}_{i}")
            ACT.op(ACT.e.activation(out=T["junk"][:], in_=T[f"xt{i}"][:], func=AF.Square, accum_out=T["ss"][:, i:i + 1]), ev=f"ssq{G}_{i}")
        ACT.wait("var" + G)
        ACT.op(ACT.e.activation(out=T["ss"][:, 4:8], in_=T["ss"][:, 4:8], func=AF.Sqrt), ev="sqrt" + G)
        for i in range(4):
            ACT.wait(f"tp{G}_{i}")
            ACT.op(ACT.e.copy(out=T["hT"][:, 0:8, i * 128:(i + 1) * 128], in_=T[f"pt{i % 2}"][:, 0:8, :]), ev=f"hTa{G}_{i}")

    def dve(T):
        DVE.wait(f"ssq{G}_3")
        DVE.op(DVE.e.tensor_scalar(out=T["ss"][:, 4:8], in0=T["ss"][:, 0:4], scalar1=1.0 / D, scalar2=EPS, op0=ALU.mult, op1=ALU.add), ev="var" + G)
        DVE.wait("sqrt" + G)
        DVE.op(DVE.e.reciprocal(out=T["ss"][:, 4:8], in_=T["ss"][:, 4:8]))
        DVE.dep()
        DVE.wait("ab_ld" + G)

        def evac(i):
            DVE.wait(f"tp{G}_{i}")
            DVE.op(DVE.e.tensor_copy(out=T["hT"][:, 8:16, i * 128:(i + 1) * 128], in_=T[f"pt{i % 2}"][:, 8:16, :]), ev=f"hTb{G}_{i}")
        for i in range(4):
            DVE.op(DVE.e.scalar_tensor_tensor(out=T["tmpf"][:], in0=T[f"xt{i}"][:], scalar=T["ss"][:, 4 + i:5 + i], in1=T["A1b"][:], op0=ALU.mult, op1=ALU.mult))
            DVE.dep()
            if i >= 2:
                DVE.wait(f"tp{G}_{i - 2}")
            DVE.op(DVE.e.tensor_tensor(out=T[f"htok{i % 2}"][:], in0=T["tmpf"][:], in1=T["B1b"][:], op=ALU.add), ev=f"htok{G}_{i}")
            if i >= 1:
                evac(i - 1)
        evac(3)

    def pe(T):
        PE.wait("kb_all")
        for i in range(4):
            PE.wait(f"htok{G}_{i}")
            if i >= 2:
                PE.wait(f"hTa{G}_{i - 2}", f"hTb{G}_{i - 2}")
            for k in range(16):
                mm = PE.e.transpose(out=T[f"pt{i % 2}"][:, k, :], in_=T[f"htok{i % 2}"][:, k * 128:(k + 1) * 128], identity=T["identb"][:])
            PE.op(mm, ev=f"tp{G}_{i}")

    cx.phase("p1", specs, {"sp": sp, "act": act, "dve": dve, "pe": pe})


IN_SPECS = [
    ("xs", [NTOK, D], F32), ("xhalo", [12, D], F32), ("hmask", [128, 12], F32), ("cvec", [3, D], F32),
    ("w_ada", [D, 6 * D], F32), ("b_ada", [1, 6 * D], F32), ("g_pre_mix", [1, D], F32), ("w_in", [D, 5120], F32),
    ("conv_w", [3, 1024], F32), ("ln_v_g", [1, 1024], F32), ("ln_v_b", [1, 1024], F32), ("w_spatial", [8, 128, 128], F32),
    ("b_spatial", [1, 1024], F32), ("g_out_a", [1, 1024], F32), ("g_out_b", [1, 1024], F32), ("w_out", [D, D], F32),
    ("g_post_mix", [1, D], F32), ("g_pre_ffn", [1, D], F32), ("w_router", [D, NE], F32), ("router_bias", [1, NE], F32),
    ("w_exp_gate", [NE, D, FF], F32), ("w_exp_up", [NE, D, FF], F32), ("w_exp_down", [NE, FF, D], F32),
    ("w_sh_gate", [D, FF], F32), ("w_sh_up", [D, FF], F32), ("w_sh_down", [FF, D], F32), ("g_post_ffn", [1, D], F32),
    ("ident", [128, 128], F32), ("tri", [128, 128], F32), ("ecap", [1, NE], F32),
]

PERSIST_G = [
    ("identf", [128, 128], F32), ("identb", [128, 128], BF16), ("trib", [128, 128], BF16), ("onesb", [128, 128], BF16), ("zerob", [128, 16], BF16),
    ("rbias", [128, NE], F32), ("ecap", [128, NE], F32),
    ("LG", [128, NT, NE], F32), ("desti", [128, NT * 8], I32), ("destA", [128, NT * 8], I32), ("destB", [128, NT * 8], I32),
    ("gatek", [128, NT * 8], F32), ("cnti", [128, NE], I32),
]
PERSIST_A = [
    ("wsT", [128, 8, 128], BF16), ("bsb", [128, 1024], F32), ("lngb", [128, 1024], F32), ("lnbb", [128, 1024], F32),
    ("convw", [128, 3, 8], F32), ("goa", [128, 8], F32), ("gob", [128, 8], F32), ("wr", [128, 16, NE], F32),
    ("hmask", [128, 12], F32), ("hTh", [128, 16, 12], BF16), ("zhalo", [128, 8, 12], F32),
    ("hT", [128, 16, GRP], BF16), ("mixin", [128, 16, GRP], BF16), ("rstdab", [128, 8], F32),
]


def build_program(debug=None):
    nc = bass.Bass("TRN2", target_bir_lowering=False)
    dr = {}
    for name, shape, dt in IN_SPECS:
        if debug in ("p1", "p2", "q", "r") and name.startswith("w_exp"):
            continue
        dr[name] = nc.dram_tensor(name, shape, dt, kind="ExternalInput").ap()
    dr["y"] = nc.dram_tensor("y", [NTOK, D], F32, kind="ExternalOutput").ap()
    dr["modv"] = nc.dram_tensor("modv", [6, 3, D], F32, kind="Internal").ap()
    dr["x1"] = nc.dram_tensor("x1s", [NTOK, D], F32, kind="Internal").ap()
    dr["h2"] = nc.dram_tensor("h2s", [NTOK, D], BF16, kind="Internal").ap()
    dr["xsl"] = nc.dram_tensor("xsl", [NE * CAP, D], BF16, kind="Internal").ap()
    dr["ysl0"] = nc.dram_tensor("ysl0", [NE // 2 * CAP, D], F32, kind="Internal").ap()
    dr["ysl1"] = nc.dram_tensor("ysl1", [NE // 2 * CAP, D], F32, kind="Internal").ap()
    dr["ysh"] = nc.dram_tensor("ysh", [NTOK, D], F32, kind="Internal").ap()
    if debug == "r":
        dr["dbg_desti"] = nc.dram_tensor("dbg_desti", [128, NT * 8], I32, kind="ExternalOutput").ap()
        dr["dbg_destA"] = nc.dram_tensor("dbg_destA", [128, NT * 8], I32, kind="ExternalOutput").ap()
        dr["dbg_destB"] = nc.dram_tensor("dbg_destB", [128, NT * 8], I32, kind="ExternalOutput").ap()
        dr["dbg_gatek"] = nc.dram_tensor("dbg_gatek", [128, NT * 8], F32, kind="ExternalOutput").ap()
        dr["dbg_LG"] = nc.dram_tensor("dbg_LG", [128, NT, NE], F32, kind="ExternalOutput").ap()
    elif debug:
        dr["dbg_hT"] = nc.dram_tensor("dbg_hT", [128, 16, GRP], F32, kind="ExternalOutput").ap()
        dr["dbg_mod"] = nc.dram_tensor("dbg_mod", [6, 3, D], F32, kind="ExternalOutput").ap()
        dr["dbg_hTh"] = nc.dram_tensor("dbg_hTh", [128, 16, 12], F32, kind="ExternalOutput").ap()
        dr["dbg_mixin"] = nc.dram_tensor("dbg_mixin", [128, 16, GRP], F32, kind="ExternalOutput").ap()
        dr["dbg_rstdab"] = nc.dram_tensor("dbg_rstdab", [128, 8], F32, kind="ExternalOutput").ap()
        dr["dbg_zhalo"] = nc.dram_tensor("dbg_zhalo", [128, 8, 12], F32, kind="ExternalOutput").ap()
        dr["dbg_LG"] = nc.dram_tensor("dbg_LG", [128, NT, NE], F32, kind="ExternalOutput").ap()
        dr["dbg_x1"] = nc.dram_tensor("dbg_x1", [1024, D], F32, kind="ExternalOutput").ap()
        dr["dbg_h2"] = nc.dram_tensor("dbg_h2", [1024, D], BF16, kind="ExternalOutput").ap()
    with ExitStack() as stack:
        sy = Sync(nc, stack)
        cx = Ctx(nc, sy, dr)
        for real in (False, True):
            sy.reset_pass(dry=not real)
            cx.uid = 0
            with ExitStack() as pst:
                cx.P = cx.alloc(pst, PERSIST_G)
                emit_all(cx, debug)
    return nc


def phase_debug_dump(cx, what):
    S = cx.S
    dr = cx.dr
    SP, DVE = S["sp"], S["dve"]
    specs = [("f", [128, 16, GRP], F32), ("f2", [128, 16, 12], F32), ("f3", [128, 16, GRP], F32)]

    def dve(T):
        DVE.op(DVE.e.tensor_copy(out=T["f"][:], in_=T["hT"][:]))
        DVE.op(DVE.e.tensor_copy(out=T["f3"][:], in_=T["mixin"][:]))
        DVE.op(DVE.e.tensor_copy(out=T["f2"][:], in_=T["hTh"][:]), ev="dbg_cp")

    def sp(T):
        SP.wait("dbg_cp")
        SP.dma("dbg", dr["dbg_hT"], T["f"][:])
        SP.dma("dbg", dr["dbg_mixin"], T["f3"][:])
        SP.dma("dbg", dr["dbg_hTh"], T["f2"][:])
        SP.dma("dbg", dr["dbg_rstdab"], T["rstdab"][:])
        SP.dma("dbg", dr["dbg_zhalo"], T["zhalo"][:])
        SP.dma("dbg", dr["dbg_LG"], T["LG"][:])
        SP.dma("dbg", dr["dbg_x1"], dr["x1"][0:1024, :])
        SP.dma("dbg", dr["dbg_h2"], dr["h2"][0:1024, :])
        SP.dma("dbg", dr["dbg_mod"], dr["modv"], ev="dbg_st")
        SP.wait("dbg_st")
    cx.phase("dbg", specs, {"sp": sp, "dve": dve})


def emit_all(cx, debug):
    PG = dict(cx.P)
    with ExitStack() as ast:
        cx.P = dict(PG)
        cx.P.update(cx.alloc(ast, PERSIST_A))
        phase0(cx)
        phase0b(cx)
        ngroups = 2 if debug in ("p1", "p2", "q") else NG
        for g in range(ngroups):
            phaseP1(cx, g)
            if debug in ("p1",):
                continue
            phaseP2(cx, g)
            if debug in ("p2",):
                continue
            phaseQ(cx, g)
        if debug in ("p1", "p2", "q"):
            phase_debug_dump(cx, debug)
            return
    cx.P = dict(PG)
    phaseR(cx)
    phaseD(cx)
    if debug == "r":
        phase_debug_r(cx)
        return
    phaseM(cx)
    phaseF(cx)


def phase_debug_r(cx):
    S = cx.S
    dr = cx.dr
    SP = S["sp"]

    def sp(T):
        SP.dma("dbg", dr["dbg_desti"], T["desti"][:])
        SP.dma("dbg", dr["dbg_destA"], T["destA"][:])
        SP.dma("dbg", dr["dbg_destB"], T["destB"][:])
        SP.dma("dbg", dr["dbg_gatek"], T["gatek"][:])
        SP.dma("dbg", dr["dbg_LG"], T["LG"][:], ev="dbgr_st")
        SP.wait("dbgr_st")
    cx.phase("dbgr", [], {"sp": sp})


def make_inputs_for_core(c, inputs, skip=()):
    xp, xsm = inputs["x_prompt"], inputs["x_sample"]
    seqs = [xp[0], xp[1], xsm[0]]
    lo = c * SEGT
    xs = np.concatenate([sq[lo:lo + SEGT] for sq in seqs], axis=0)
    xhalo = np.zeros((12, D), np.float32)
    hmask = np.zeros((128, 12), np.float32)
    for g in range(NG):
        s, half = g // 2, g % 2
        st = lo + half * GRP
        if st - 1 >= 0:
            xhalo[2 * g] = seqs[s][st - 1]
            hmask[:, 2 * g] = 1.0
        if st + GRP < seqs[s].shape[0]:
            xhalo[2 * g + 1] = seqs[s][st + GRP]
            hmask[:, 2 * g + 1] = 1.0
    cvec = np.stack([inputs["c_prompt"][0], inputs["c_prompt"][1], inputs["c_sample"][0]], axis=0)
    m = {"xs": np.ascontiguousarray(xs), "xhalo": xhalo, "hmask": hmask, "cvec": np.ascontiguousarray(cvec)}
    for name, shape, dt in IN_SPECS:
        if name in m or name in ("ident", "tri", "ecap") or name in skip:
            continue
        m[name] = np.ascontiguousarray(np.asarray(inputs[name])[0]).reshape(shape)
    m["ident"] = np.eye(128, dtype=np.float32)
    m["tri"] = np.triu(np.ones((128, 128), np.float32), k=1)
    m["ecap"] = (np.arange(NE, dtype=np.float32) * CAP).reshape(1, NE)
    return m


def phaseP2(cx, g):
    S = cx.S
    dr = cx.dr
    SP, PL, ACT, DVE, PE = S["sp"], S["pool"], S["act"], S["dve"], S["pe"]
    G = f"{g}"
    NWB = 4
    specs = [(f"w{i}", [128, 16, 256], BF16) for i in range(NWB)] + [
        ("t1", [128, GRP], F32), ("z", [128, GRP + 2], F32), ("c0", [128, GRP], F32), ("c1", [128, GRP], F32),
        ("raw0", [128, GRP], F32), ("raw1", [128, GRP], F32), ("sq0", [128, GRP], BF16), ("sq1", [128, GRP], BF16),
        ("ug", [128, 8, GRP], BF16),
        ("vg0", [128, 1024], F32), ("vg1", [128, 1024], F32), ("vg2", [128, 1024], F32), ("vg3", [128, 1024], F32),
        ("vsq0", [128, 1024], F32), ("vsq1", [128, 1024], F32), ("vsq2", [128, 1024], F32), ("vsq3", [128, 1024], F32), ("vn", [128, 1024], F32),
        ("vnb0", [128, 1024], BF16), ("vnb1", [128, 1024], BF16), ("vnb2", [128, 1024], BF16), ("vnb3", [128, 1024], BF16),
        ("vs", [128, 4, 32], F32), ("spt", [128, GRP], F32), ("zh", [128, 12], F32), ("stt", [128, 8], F32),
        ("pA", [128, 512], F32, "psum"), ("pB", [128, 512], F32, "psum"), ("pC", [128, 512], F32, "psum"),
        ("pD", [128, 512], F32, "psum"), ("pE", [128, 512], F32, "psum"), ("pF", [128, 512], F32, "psum"),
        ("pst", [128, 512], F32, "psum"), ("ph", [128, 2, 12], F32, "psum"),
    ]
    blocks = []
    for vj in range(4):
        blocks.append(("v", vj, 4096 + vj * 256))
    for cj in range(4):
        blocks += [("cg", cj, 1024 + cj * 256), ("xh", cj, 2048 + cj * 256), ("bg", cj, cj * 256)]
    for uj in range(4):
        blocks.append(("u", uj, 3072 + uj * 256))
    bidx = {(n, j): i for i, (n, j, c) in enumerate(blocks)}

    def wld(n, j):
        return f"w_ld{G}_{bidx[(n, j)]}"

    def wbuf(T, n, j):
        return T[f"w{bidx[(n, j)] % NWB}"]

    def pool(T):
        for i, (n, j, c0) in enumerate(blocks):
            if i >= NWB:
                PL.wait(f"w_free{G}_{i - NWB}")
            PL.dma(f"w{i % NWB}", T[f"w{i % NWB}"][:], dr["w_in"][:, c0:c0 + 256].rearrange("(k p) n -> p k n", p=128), ev=f"w_ld{G}_{i}")

    cbanks = [("pA", "pB", "pC"), ("pD", "pE", "pF")]

    def pe(T):
        hT = T["hT"]
        def stats(c, br):
            PE.wait(f"sq{br}{G}_{c}")
            sq = T[f"sq{c % 2}"]
            for i in range(4):
                col = (0 if br == "a" else 4) + i
                mm = PE.e.matmul(T["pst"][:, col:col + 1], sq[:, i * 128:(i + 1) * 128], T["onesb"][:, 0:1], start=False, stop=(c == 7), skip_group_check=True)
            PE.op(mm, ev=f"st{br}{G}_{c}")

        PE.wait("ones_set")
        PE.e.matmul(T["pst"][:, 0:8], T["onesb"][:, :], T["zerob"][:, 0:8], start=True, stop=False, skip_group_check=True)
        vbanks = ["pE", "pF"]
        n_v = 0
        for vj in range(4):
            PE.wait(wld("v", vj))
            W = wbuf(T, "v", vj)
            for i in range(4):
                bank = vbanks[n_v % 2]
                if n_v >= 2:
                    PE.wait(f"vg{G}_{n_v - 2}")
                for k in range(16):
                    mm = PE.e.matmul(T[bank][:, 0:256], hT[:, k, i * 128:(i + 1) * 128], W[:, k, :], start=(k == 0), stop=(k == 15))
                PE.op(mm, ev=f"pj_v{G}_{n_v}")
                n_v += 1
            S["pe"]._rec(f"w_free{G}_{bidx[('v', vj)]}", S["pe"].semname, cx.sy.counts[S["pe"].semname])
        for c in range(8):
            cj, cc = c // 2, c % 2
            bk = cbanks[c % 2]
            for n, bank in (("cg", bk[0]), ("xh", bk[1]), ("bg", bk[2])):
                PE.wait(wld(n, cj))
                if c >= 2:
                    PE.wait(f"ev_{n}{G}_{c - 2}")
                elif c == 1:
                    PE.wait(f"vg{G}_14", f"vg{G}_15")
                W = wbuf(T, n, cj)
                for k in range(16):
                    mm = PE.e.matmul(T[bank][:, :], W[:, k, cc * 128:(cc + 1) * 128], hT[:, k, :], start=(k == 0), stop=(k == 15))
                    if g == 0 and n in ("cg", "xh"):
                        hi = 0 if n == "cg" else 1
                        if k == 0 and c >= 1:
                            PE.wait(f"zh{G}_{c - 1}")
                        mm = PE.e.matmul(T["ph"][:, hi, :], W[:, k, cc * 128:(cc + 1) * 128], T["hTh"][:, k, :], start=(k == 0), stop=(k == 15), skip_group_check=True)
                PE.op(mm, ev=f"pj_{n}{G}_{c}")
                if cc == 1:
                    S["pe"]._rec(f"w_free{G}_{bidx[(n, cj)]}", S["pe"].semname, cx.sy.counts[S["pe"].semname])
            if c >= 1:
                stats(c - 1, "a")
        stats(7, "a")
        ubanks = ["pA", "pB", "pC", "pD"]
        for c in range(8):
            uj, cc = c // 2, c % 2
            PE.wait(wld("u", uj))
            bank = ubanks[c % 4]
            if c >= 4:
                PE.wait(f"ug{G}_{c - 4}")
            else:
                PE.wait(f"ev_cg{G}_{7}", f"ev_xh{G}_{7}", f"ev_bg{G}_{7}", f"ev_cg{G}_{6}", f"ev_xh{G}_{6}", f"ev_bg{G}_{6}")
            W = wbuf(T, "u", uj)
            for k in range(16):
                mm = PE.e.matmul(T[bank][:, :], W[:, k, cc * 128:(cc + 1) * 128], hT[:, k, :], start=(k == 0), stop=(k == 15))
            PE.op(mm, ev=f"pj_u{G}_{c}")
            if cc == 1:
                S["pe"]._rec(f"w_free{G}_{bidx[('u', uj)]}", S["pe"].semname, cx.sy.counts[S["pe"].semname])
        sbanks = ["pA", "pB"]
        for h in range(8):
            bank = sbanks[h % 2]
            if h >= 2:
                PE.wait(f"spt{G}_{h - 2}")
            else:
                PE.wait(f"ug{G}_{7}", f"ug{G}_{6}", f"ug{G}_{5}", f"ug{G}_{4}")
            for i in range(4):
                PE.wait(f"vnb{G}_{i}")
                mm = PE.e.matmul(T[bank][:, i * 128:(i + 1) * 128], T[f"vnb{i}"][:, h * 128:(h + 1) * 128], T["wsT"][:, h, :], start=True, stop=True)
            PE.op(mm, ev=f"pj_s{G}_{h}")
            if h >= 1:
                stats(h - 1, "b")
        stats(7, "b")

    def act(T):
        vbanks = ["pE", "pF"]
        n_v = 0
        for vj in range(4):
            for i in range(4):
                ACT.wait(f"pj_v{G}_{n_v}")
                ACT.op(ACT.e.activation(out=T[f"vg{i}"][:, vj * 256:(vj + 1) * 256], in_=T[vbanks[n_v % 2]][:, 0:256], func=AF.Gelu), ev=f"vg{G}_{n_v}")
                n_v += 1
        ACT.dep()
        for i in range(4):
            ACT.op(ACT.e.activation(out=T[f"vsq{i}"][:], in_=T[f"vg{i}"][:], func=AF.Square), ev=f"vsq{G}_{i}")
        ACT.wait(f"vvar{G}")
        ACT.op(ACT.e.activation(out=T["vs"][:, :, 8:16], in_=T["vs"][:, :, 8:16], func=AF.Sqrt), ev=f"vsqrt{G}")
        for c in range(8):
            bk = cbanks[c % 2]
            ACT.wait(f"pj_cg{G}_{c}")
            if c >= 1:
                ACT.wait(f"z{G}_{c - 1}")
            ACT.op(ACT.e.copy(out=T["t1"][:], in_=T[bk[0]][:, :]), ev=f"ev_cg{G}_{c}")
            if c >= 1:
                sqmix(T, c - 1, "a")
        sqmix(T, 7, "a")
        ubanks = ["pA", "pB", "pC", "pD"]
        for c in range(8):
            ACT.wait(f"pj_u{G}_{c}")
            ACT.op(ACT.e.activation(out=T["ug"][:, c, :], in_=T[ubanks[c % 4]][:, :], func=AF.Gelu), ev=f"ug{G}_{c}")
        for h in range(8):
            sqmix(T, h, "b")
        ACT.wait(f"stv{G}")
        ACT.op(ACT.e.activation(out=T["stt"][:], in_=T["stt"][:], func=AF.Sqrt), ev=f"stsq{G}")

    def sqmix(T, c, br):
        ACT.wait(f"raw{br}{G}_{c}")
        if c >= 2:
            ACT.wait(f"st{br}{G}_{c - 2}")
        elif br == "b":
            ACT.wait(f"sta{G}_{6 + c}")
        raw = T[f"raw{c % 2}"]
        ACT.op(ACT.e.activation(out=T[f"sq{c % 2}"][:], in_=raw[:], func=AF.Square), ev=f"sq{br}{G}_{c}")
        gn = T["goa"] if br == "a" else T["gob"]
        cm = c if br == "a" else 8 + c
        ACT.op(ACT.e.activation(out=T["mixin"][:, cm, :], in_=raw[:], func=AF.Copy, scale=gn[:, c:c + 1]), ev=f"mix{br}{G}_{c}")

    def dve(T):
        vs = T["vs"]
        for i in range(4):
            DVE.wait(f"vg{G}_{12 + i}")
            DVE.op(DVE.e.tensor_reduce(out=vs[:, i, 0:8], in_=T[f"vg{i}"][:].rearrange("p (h d) -> p h d", h=8), axis=AX.X, op=ALU.add))
            DVE.wait(f"vsq{G}_{i}")
            DVE.op(DVE.e.tensor_reduce(out=vs[:, i, 8:16], in_=T[f"vsq{i}"][:].rearrange("p (h d) -> p h d", h=8), axis=AX.X, op=ALU.add), ev=f"vs2{G}_{i}")
        DVE.dep()
        DVE.op(DVE.e.tensor_scalar(out=vs[:, :, 0:8], in0=vs[:, :, 0:8], scalar1=1.0 / 128, scalar2=None, op0=ALU.mult))
        DVE.dep()
        DVE.op(DVE.e.tensor_tensor(out=vs[:, :, 16:24], in0=vs[:, :, 0:8], in1=vs[:, :, 0:8], op=ALU.mult))
        DVE.op(DVE.e.tensor_scalar(out=vs[:, :, 8:16], in0=vs[:, :, 8:16], scalar1=1.0 / 128, scalar2=EPS, op0=ALU.mult, op1=ALU.add))
        DVE.dep()
        DVE.op(DVE.e.tensor_tensor(out=vs[:, :, 8:16], in0=vs[:, :, 8:16], in1=vs[:, :, 16:24], op=ALU.subtract), ev=f"vvar{G}")
        DVE.wait(f"vsqrt{G}")
        DVE.op(DVE.e.reciprocal(out=vs[:, :, 8:16], in_=vs[:, :, 8:16]))
        DVE.dep()
        for i in range(4):
            v3 = T[f"vg{i}"][:].rearrange("p (h d) -> p h d", h=8)
            n3 = T["vn"][:].rearrange("p (h d) -> p h d", h=8)
            DVE.op(DVE.e.tensor_tensor(out=n3, in0=v3, in1=bcast3(vs[:, i, 0:8], 8, 128), op=ALU.subtract))
            DVE.dep()
            DVE.op(DVE.e.tensor_tensor(out=n3, in0=n3, in1=bcast3(vs[:, i, 8:16], 8, 128), op=ALU.mult))
            DVE.dep()
            DVE.op(DVE.e.tensor_tensor(out=T["vn"][:], in0=T["vn"][:], in1=T["lngb"][:], op=ALU.mult))
            DVE.dep()
            DVE.op(DVE.e.tensor_tensor(out=T[f"vnb{i}"][:], in0=T["vn"][:], in1=T["lnbb"][:], op=ALU.add), ev=f"vnb{G}_{i}")
        z = T["z"]
        cw = T["convw"]
        for c in range(8):
            bk = cbanks[c % 2]
            if g == 0:
                DVE.wait(f"pj_xh{G}_{c}")
                DVE.op(DVE.e.tensor_copy(out=T["zh"][:], in_=T["ph"][:, 0, :]))
                DVE.dep()
                DVE.op(DVE.e.tensor_tensor(out=T["zh"][:], in0=T["zh"][:], in1=T["ph"][:, 1, :], op=ALU.mult), ev=f"zh{G}_{c}")
                DVE.dep()
                DVE.op(DVE.e.tensor_tensor(out=T["zhalo"][:, c, :], in0=T["zh"][:], in1=T["hmask"][:], op=ALU.mult))
                DVE.dep()
            DVE.wait(f"ev_cg{G}_{c}", f"pj_xh{G}_{c}")
            DVE.op(DVE.e.tensor_tensor(out=z[:, 1:GRP + 1], in0=T["t1"][:], in1=T[bk[1]][:, :], op=ALU.mult), ev=f"z{G}_{c}")
            S["dve"]._rec(f"ev_xh{G}_{c}", S["dve"].semname, cx.sy.counts[S["dve"].semname])
            DVE.op(DVE.e.tensor_copy(out=z[:, 0:1], in_=T["zhalo"][:, c, 2 * g:2 * g + 1]))
            DVE.op(DVE.e.tensor_copy(out=z[:, GRP + 1:GRP + 2], in_=T["zhalo"][:, c, 2 * g + 1:2 * g + 2]))
            DVE.dep()
            DVE.op(DVE.e.tensor_scalar(out=T["c0"][:], in0=z[:, 0:GRP], scalar1=cw[:, 0, c:c + 1], scalar2=None, op0=ALU.mult))
            DVE.dep()
            DVE.op(DVE.e.scalar_tensor_tensor(out=T["c1"][:], in0=z[:, 1:GRP + 1], scalar=cw[:, 1, c:c + 1], in1=T["c0"][:], op0=ALU.mult, op1=ALU.add))
            DVE.dep()
            DVE.op(DVE.e.scalar_tensor_tensor(out=T["c0"][:], in0=z[:, 2:GRP + 2], scalar=cw[:, 2, c:c + 1], in1=T["c1"][:], op0=ALU.mult, op1=ALU.add))
            DVE.dep()
            DVE.wait(f"pj_bg{G}_{c}")
            if c >= 2:
                DVE.wait(f"mixa{G}_{c - 2}")
            DVE.op(DVE.e.tensor_tensor(out=T[f"raw{c % 2}"][:], in0=T[bk[2]][:, :], in1=T["c0"][:], op=ALU.mult), ev=f"rawa{G}_{c}")
            S["dve"]._rec(f"ev_bg{G}_{c}", S["dve"].semname, cx.sy.counts[S["dve"].semname])
        sbanks = ["pA", "pB"]
        for h in range(8):
            DVE.wait(f"pj_s{G}_{h}")
            bsv = T["bsb"][:, h * 128:(h + 1) * 128].unsqueeze(1).to_broadcast([128, 4, 128])
            DVE.op(DVE.e.tensor_tensor(out=T["spt"][:].rearrange("p (i q) -> p i q", i=4), in0=T[sbanks[h % 2]][:, :].rearrange("p (i q) -> p i q", i=4), in1=bsv, op=ALU.add), ev=f"spt{G}_{h}")
            DVE.dep()
            if h >= 2:
                DVE.wait(f"mixb{G}_{h - 2}")
            else:
                DVE.wait(f"mixa{G}_{6 + h}")
            DVE.op(DVE.e.tensor_tensor(out=T[f"raw{h % 2}"][:], in0=T["spt"][:], in1=T["ug"][:, h, :], op=ALU.mult), ev=f"rawb{G}_{h}")
        DVE.wait(f"stb{G}_7", f"sta{G}_7")
        DVE.op(DVE.e.tensor_scalar(out=T["stt"][:], in0=T["pst"][:, 0:8], scalar1=1.0 / 1024, scalar2=EPS, op0=ALU.mult, op1=ALU.add), ev=f"stv{G}")
        DVE.wait(f"stsq{G}")
        DVE.op(DVE.e.reciprocal(out=T["rstdab"][:], in_=T["stt"][:]), ev=f"rstdab{G}")

    cx.phase("p2", specs, {"pool": pool, "act": act, "dve": dve, "pe": pe})


def phaseQ(cx, g):
    S = cx.S
    dr = cx.dr
    SP, PL, ACT, DVE, PE = S["sp"], S["pool"], S["act"], S["dve"], S["pe"]
    G = f"{g}"
    s = g // 2
    specs = [("wo0", [128, 16, 512], BF16), ("wo1", [128, 16, 512], BF16)] + [(f"mix{i}", [128, D], F32) for i in range(4)] + [
        ("xt0", [128, D], F32), ("xt1", [128, D], F32), ("G1b", [128, D], F32), ("A2b", [128, D], F32), ("B2b", [128, D], F32),
        ("junk", [128, D], BF16), ("h2b0", [128, D], BF16), ("h2b1", [128, D], BF16), ("h2T", [128, 16, 128], F32),
        ("ss", [128, 16], F32),
        ("pa0", [128, 512], F32, "psum"), ("pb0", [128, 512], F32, "psum"), ("pa1", [128, 512], F32, "psum"), ("pb1", [128, 512], F32, "psum"),
        ("ptr0", [128, 4, 128], F32, "psum"), ("ptr1", [128, 4, 128], F32, "psum"), ("plg", [128, 512], F32, "psum"),
    ]

    def pool(T):
        for ob in range(4):
            if ob >= 2:
                PL.wait(f"wo_free{G}_{ob - 2}")
            PL.dma(f"wo{ob % 2}", T[f"wo{ob % 2}"][:], dr["w_out"][:, ob * 512:(ob + 1) * 512].rearrange("(k p) n -> p k n", p=128), ev=f"wo_ld{G}_{ob}")

    def sp(T):
        SP.dma("qb", T["G1b"][:], dr["modv"][1, s:s + 1, :].partition_broadcast(128))
        SP.dma("qb", T["A2b"][:], dr["modv"][2, s:s + 1, :].partition_broadcast(128))
        SP.dma("qb", T["B2b"][:], dr["modv"][5, s:s + 1, :].partition_broadcast(128), ev="qb_ld" + G)
        for i in range(4):
            r0 = g * GRP + i * 128
            if i >= 2:
                SP.wait(f"x1st{G}_{i - 2}", f"h2f{G}_{i - 2}")
            SP.dma(f"qx{i % 2}", T[f"xt{i % 2}"][:], dr["xs"][r0:r0 + 128, :], ev=f"qx_ld{G}_{i}")
            if i >= 1:
                st(T, i - 1)
        st(T, 3)
        SP.wait(f"x1st{G}_2", f"h2st{G}_2", f"x1st{G}_3", f"h2st{G}_3")

    def st(T, i):
        r0 = g * GRP + i * 128
        SP.wait(f"x1{G}_{i}")
        SP.dma(f"qs{i % 2}", dr["x1"][r0:r0 + 128, :], T[f"xt{i % 2}"][:], ev=f"x1st{G}_{i}")
        SP.wait(f"h2b{G}_{i}")
        SP.dma(f"qh{i % 2}", dr["h2"][r0:r0 + 128, :], T[f"h2b{i % 2}"][:], ev=f"h2st{G}_{i}")

    def pe(T):
        mixin = T["mixin"]
        n = 0
        for ob in range(4):
            PE.wait(f"wo_ld{G}_{ob}")
            W = T[f"wo{ob % 2}"]
            for i in range(4):
                pa, pb = T[f"pa{n % 2}"], T[f"pb{n % 2}"]
                if n >= 2:
                    PE.wait(f"mixev{G}_{n - 2}")
                for c in range(8):
                    mm = PE.e.matmul(pa[:, :], mixin[:, c, i * 128:(i + 1) * 128], W[:, c, :], start=(c == 0), stop=(c == 7))
                for c in range(8, 16):
                    mm = PE.e.matmul(pb[:, :], mixin[:, c, i * 128:(i + 1) * 128], W[:, c, :], start=(c == 8), stop=(c == 15))
                PE.op(mm, ev=f"mm{G}_{n}")
                n += 1
            S["pe"]._rec(f"wo_free{G}_{ob}", S["pe"].semname, cx.sy.counts[S["pe"].semname])
        for i in range(4):
            PE.wait(f"h2f{G}_{i}")
            for q in range(4):
                ptr = T[f"ptr{q % 2}"]
                if i * 4 + q >= 2:
                    PE.wait(f"h2Tev{G}_{i * 4 + q - 2}")
                for kk in range(4):
                    k = q * 4 + kk
                    mm = PE.e.transpose(out=ptr[:, kk, :], in_=T[f"mix{i}"][:, k * 128:(k + 1) * 128], identity=T["identf"][:])
                PE.op(mm, ev=f"h2Tpe{G}_{i * 4 + q}")
            PE.wait(f"h2Tev{G}_{i * 4 + 2}", f"h2Tev{G}_{i * 4 + 3}")
            if i >= 1:
                PE.wait(f"lgev{G}_{i - 1}")
            for k in range(16):
                mm = PE.e.matmul(T["plg"][:, 0:NE], T["h2T"][:, k, :], T["wr"][:, k, :], start=(k == 0), stop=(k == 15))
            PE.op(mm, ev=f"lgpe{G}_{i}")

    def act(T):
        for i in range(4):
            ACT.wait(f"mixev{G}_{12 + i}")
            ACT.op(ACT.e.activation(out=T["junk"][:], in_=T[f"mix{i}"][:], func=AF.Square, accum_out=T["ss"][:, i:i + 1]), ev=f"qss{G}_{i}")
        ACT.wait(f"qvar{G}")
        ACT.op(ACT.e.activation(out=T["ss"][:, 4:8], in_=T["ss"][:, 4:8], func=AF.Sqrt), ev=f"qsqrt{G}")
        for i in range(4):
            ACT.wait(f"x1{G}_{i}")
            ACT.op(ACT.e.activation(out=T["junk"][:], in_=T[f"xt{i % 2}"][:], func=AF.Square, accum_out=T["ss"][:, 8 + i:9 + i]), ev=f"qss2{G}_{i}")
            ACT.wait(f"qvar2{G}_{i}")
            ACT.op(ACT.e.activation(out=T["ss"][:, 12 + i:13 + i], in_=T["ss"][:, 12 + i:13 + i], func=AF.Sqrt), ev=f"qsqrt2{G}_{i}")
            for q in (0, 2):
                ACT.wait(f"h2Tpe{G}_{i * 4 + q}")
                ACT.op(ACT.e.copy(out=T["h2T"][:, q * 4:q * 4 + 4, :], in_=T[f"ptr{q % 2}"][:, :, :]), ev=f"h2Tev{G}_{i * 4 + q}")

    def dve(T):
        ra, rb = T["rstdab"][:, 0:4], T["rstdab"][:, 4:8]
        n = 0
        for ob in range(4):
            for i in range(4):
                DVE.wait(f"mm{G}_{n}")
                msl = T[f"mix{i}"][:, ob * 512:(ob + 1) * 512]
                DVE.op(DVE.e.tensor_scalar(out=msl, in0=T[f"pa{n % 2}"][:, :], scalar1=ra[:, i:i + 1], scalar2=None, op0=ALU.mult))
                DVE.dep()
                DVE.op(DVE.e.scalar_tensor_tensor(out=msl, in0=T[f"pb{n % 2}"][:, :], scalar=rb[:, i:i + 1], in1=msl, op0=ALU.mult, op1=ALU.add), ev=f"mixev{G}_{n}")
                n += 1
        ss = T["ss"]
        DVE.wait(f"qss{G}_3")
        DVE.op(DVE.e.tensor_scalar(out=ss[:, 4:8], in0=ss[:, 0:4], scalar1=1.0 / D, scalar2=EPS, op0=ALU.mult, op1=ALU.add), ev=f"qvar{G}")
        DVE.wait(f"qsqrt{G}")
        DVE.op(DVE.e.reciprocal(out=ss[:, 4:8], in_=ss[:, 4:8]))
        DVE.dep()
        DVE.wait("qb_ld" + G)
        for i in range(4):
            xt = T[f"xt{i % 2}"]
            mix = T[f"mix{i}"]
            DVE.wait(f"qx_ld{G}_{i}")
            DVE.op(DVE.e.scalar_tensor_tensor(out=mix[:], in0=mix[:], scalar=ss[:, 4 + i:5 + i], in1=T["G1b"][:], op0=ALU.mult, op1=ALU.mult))
            DVE.dep()
            DVE.op(DVE.e.tensor_tensor(out=xt[:], in0=xt[:], in1=mix[:], op=ALU.add), ev=f"x1{G}_{i}")
            DVE.wait(f"qss2{G}_{i}")
            DVE.op(DVE.e.tensor_scalar(out=ss[:, 12 + i:13 + i], in0=ss[:, 8 + i:9 + i], scalar1=1.0 / D, scalar2=EPS, op0=ALU.mult, op1=ALU.add), ev=f"qvar2{G}_{i}")
            DVE.wait(f"qsqrt2{G}_{i}")
            DVE.op(DVE.e.reciprocal(out=ss[:, 12 + i:13 + i], in_=ss[:, 12 + i:13 + i]))
            DVE.dep()
            DVE.op(DVE.e.scalar_tensor_tensor(out=mix[:], in0=xt[:], scalar=ss[:, 12 + i:13 + i], in1=T["A2b"][:], op0=ALU.mult, op1=ALU.mult))
            DVE.dep()
            DVE.op(DVE.e.tensor_tensor(out=mix[:], in0=mix[:], in1=T["B2b"][:], op=ALU.add), ev=f"h2f{G}_{i}")
            DVE.dep()
            if i >= 2:
                DVE.wait(f"h2st{G}_{i - 2}")
            DVE.op(DVE.e.tensor_copy(out=T[f"h2b{i % 2}"][:], in_=mix[:]), ev=f"h2b{G}_{i}")
            for q in (1, 3):
                DVE.wait(f"h2Tpe{G}_{i * 4 + q}")
                DVE.op(DVE.e.tensor_copy(out=T["h2T"][:, q * 4:q * 4 + 4, :], in_=T[f"ptr{q % 2}"][:, :, :]), ev=f"h2Tev{G}_{i * 4 + q}")
            DVE.wait(f"lgpe{G}_{i}")
            DVE.op(DVE.e.tensor_copy(out=T["LG"][:, g * 4 + i, :], in_=T["plg"][:, 0:NE]), ev=f"lgev{G}_{i}")

    cx.phase("q", specs, {"pool": pool, "sp": sp, "act": act, "dve": dve, "pe": pe})


def phaseR(cx):
    S = cx.S
    dr = cx.dr
    SP, PL, ACT, DVE, PE = S["sp"], S["pool"], S["act"], S["dve"], S["pe"]
    NL = NT * NE
    NGp = NT * 8
    big = ["SC", "SEL", "TMP", "EQ", "MSK", "M", "WF", "DD"]
    specs = [(n, [128, NL], F32) for n in big] + [
        ("Mb", [128, NL], BF16), ("g1", [128, NGp], F32), ("g2", [128, NGp], F32), ("gs", [128, NGp], F32), ("gm", [128, NGp], F32),
        ("geq", [128, NGp], F32), ("mx", [128, NT], F32), ("m8", [128, NT, 8], F32), ("i8", [128, NT, 8], U32),
        ("idc", [128, NGp], F32), ("sums", [128, NT], F32), ("destf", [128, NGp], F32), ("dA", [128, NGp], F32), ("dB", [128, NGp], F32),
        ("isB", [128, NGp], F32),
        ("pp0", [128, 512], F32, "psum"), ("pp1", [128, 512], F32, "psum"), ("pc", [128, 512], F32, "psum"),
    ]

    def v3(ap):
        return ap[:].rearrange("p (i e) -> p i e", e=NE)

    def v4(ap):
        return ap[:].rearrange("p (a j) -> p a j", j=8)

    def act(T):
        ACT.op(ACT.e.activation(out=T["SC"][:], in_=T["LG"][:].rearrange("p i e -> p (i e)"), func=AF.Sigmoid), ev="r_sc")

    def pe(T):
        PE.wait("r_Mb")
        for i in range(NT):
            pp = T[f"pp{i % 2}"]
            if i >= 2:
                PE.wait(f"r_dd{i - 2}")
            mm = PE.e.matmul(pp[:, 0:NE], T["trib"][:, :], T["Mb"][:, i * NE:(i + 1) * NE], start=True, stop=(i == 0))
            for j in range(i):
                mm = PE.e.matmul(pp[:, 0:NE], T["onesb"][:, :], T["Mb"][:, j * NE:(j + 1) * NE], start=False, stop=(j == i - 1))
            PE.op(mm, ev=f"r_pos{i}")
        for i in range(NT):
            mm = PE.e.matmul(T["pc"][:, 0:NE], T["onesb"][:, :], T["Mb"][:, i * NE:(i + 1) * NE], start=(i == 0), stop=(i == NT - 1))
        PE.op(mm, ev="r_cnt")

    def dve(T):
        def o(instr, ev=None):
            DVE.op(instr, ev=ev)
            DVE.dep()
        e = DVE.e
        SC, SEL, TMP, EQ, MSK, M, WF, DD = (T[n] for n in big)
        DVE.wait("r_sc")
        rb = T["rbias"][:, :].unsqueeze(1).to_broadcast([128, NT, NE])
        o(e.tensor_tensor(out=v3(SEL), in0=v3(SC), in1=rb, op=ALU.add))
        o(e.tensor_reduce(out=T["g1"][:], in_=v4(SEL), axis=AX.X, op=ALU.max))
        o(e.tensor_tensor(out=v4(EQ), in0=v4(SEL), in1=bcast3(T["g1"][:, :], NGp, 8), op=ALU.is_equal))
        o(e.scalar_tensor_tensor(out=TMP[:], in0=EQ[:], scalar=-BIG, in1=SEL[:], op0=ALU.mult, op1=ALU.add))
        o(e.tensor_reduce(out=T["g2"][:], in_=v4(TMP), axis=AX.X, op=ALU.max))
        o(e.tensor_tensor(out=T["gs"][:], in0=T["g1"][:], in1=T["g2"][:], op=ALU.add))
        o(e.memset(T["gm"][:], 0.0))
        for r in range(4):
            o(e.tensor_reduce(out=T["mx"][:], in_=v4(T["gs"]), axis=AX.X, op=ALU.max))
            o(e.tensor_tensor(out=v4(T["geq"]), in0=v4(T["gs"]), in1=bcast3(T["mx"][:, :], NT, 8), op=ALU.is_equal))
            o(e.tensor_tensor(out=T["gm"][:], in0=T["gm"][:], in1=T["geq"][:], op=ALU.add))
            o(e.scalar_tensor_tensor(out=T["gs"][:], in0=T["geq"][:], scalar=-BIG, in1=T["gs"][:], op0=ALU.mult, op1=ALU.add))
        o(e.tensor_tensor(out=v4(TMP), in0=v4(SEL), in1=bcast3(T["gm"][:, :], NGp, 8), op=ALU.mult))
        o(e.tensor_scalar(out=T["geq"][:], in0=T["gm"][:], scalar1=BIG, scalar2=-BIG, op0=ALU.mult, op1=ALU.add))
        o(e.tensor_tensor(out=v4(MSK), in0=v4(TMP), in1=bcast3(T["geq"][:, :], NGp, 8), op=ALU.add))
        for i in range(NT):
            DVE.op(e.max(out=T["m8"][:, i, :], in_=MSK[:, i * NE:(i + 1) * NE]))
        DVE.dep()
        for i in range(NT):
            DVE.op(e.max_index(out=T["i8"][:, i, :], in_max=T["m8"][:, i, :], in_values=MSK[:, i * NE:(i + 1) * NE]))
        DVE.dep()
        o(e.tensor_tensor(out=v3(M), in0=v3(MSK), in1=bcast3(T["m8"][:, :, 7], NT, NE), op=ALU.is_ge))
        o(e.tensor_copy(out=T["Mb"][:], in_=M[:]), ev="r_Mb")
        o(e.tensor_tensor(out=WF[:], in0=M[:], in1=SC[:], op=ALU.mult))
        o(e.tensor_reduce(out=T["sums"][:], in_=v3(WF), axis=AX.X, op=ALU.add))
        o(e.tensor_scalar(out=T["sums"][:], in0=T["sums"][:], scalar1=1e-20, scalar2=None, op0=ALU.add))
        o(e.reciprocal(out=T["sums"][:], in_=T["sums"][:]))
        o(e.scalar_tensor_tensor(out=v3(WF), in0=v3(WF), scalar=2.5, in1=bcast3(T["sums"][:, :], NT, NE), op0=ALU.mult, op1=ALU.mult))
        ec = T["ecap"][:, :]
        for i in range(NT):
            DVE.wait(f"r_pos{i}")
            DVE.op(e.tensor_tensor(out=DD[:, i * NE:(i + 1) * NE], in0=T[f"pp{i % 2}"][:, 0:NE], in1=ec, op=ALU.add), ev=f"r_dd{i}")
        DVE.dep()
        o(e.tensor_copy(out=T["idc"][:], in_=T["i8"][:].rearrange("p i k -> p (i k)")))
        o(e.tensor_scalar(out=T["idc"][:], in0=T["idc"][:], scalar1=float(CAP), scalar2=None, op0=ALU.mult))
        ecb = T["ecap"][:, :].unsqueeze(1).to_broadcast([128, NT, NE])
        idc3 = T["idc"][:].rearrange("p (i k) -> p i k", k=8)
        df3 = T["destf"][:].rearrange("p (i k) -> p i k", k=8)
        gk3 = T["gatek"][:].rearrange("p (i k) -> p i k", k=8)
        for k in range(8):
            o(e.tensor_tensor(out=v3(EQ), in0=ecb, in1=bcast3(idc3[:, :, k], NT, NE), op=ALU.is_equal))
            o(e.tensor_tensor(out=TMP[:], in0=EQ[:], in1=DD[:], op=ALU.mult))
            o(e.tensor_reduce(out=df3[:, :, k], in_=v3(TMP), axis=AX.X, op=ALU.add))
            o(e.tensor_tensor(out=TMP[:], in0=EQ[:], in1=WF[:], op=ALU.mult))
            o(e.tensor_reduce(out=gk3[:, :, k], in_=v3(TMP), axis=AX.X, op=ALU.add))
        half = float(NE // 2 * CAP)
        o(e.tensor_copy(out=T["desti"][:], in_=T["destf"][:]))
        o(e.tensor_scalar(out=T["isB"][:], in0=T["destf"][:], scalar1=half, scalar2=None, op0=ALU.is_ge))
        HUGE = 4.0e6
        o(e.scalar_tensor_tensor(out=T["dA"][:], in0=T["isB"][:], scalar=HUGE, in1=T["destf"][:], op0=ALU.mult, op1=ALU.add))
        o(e.tensor_scalar(out=T["dB"][:], in0=T["isB"][:], scalar1=-HUGE, scalar2=HUGE - half, op0=ALU.mult, op1=ALU.add))
        o(e.tensor_tensor(out=T["dB"][:], in0=T["dB"][:], in1=T["destf"][:], op=ALU.add))
        o(e.tensor_copy(out=T["destA"][:], in_=T["dA"][:]))
        o(e.tensor_copy(out=T["destB"][:], in_=T["dB"][:]))
        DVE.wait("r_cnt")
        o(e.tensor_copy(out=T["cnti"][:], in_=T["pc"][:, 0:NE]), ev="r_done")

    cx.phase("r", specs, {"act": act, "dve": dve, "pe": pe})


def phaseD(cx):
    S = cx.S
    dr = cx.dr
    SP, PL = S["sp"], S["pool"]
    specs = [(f"hb{i}", [128, D], BF16) for i in range(3)]

    def sp(T):
        for i in range(NT):
            if i >= 3:
                SP.wait(f"d_sc{i - 3}")
            SP.dma(f"dh{i % 3}", T[f"hb{i % 3}"][:], dr["h2"][i * 128:(i + 1) * 128, :], ev=f"d_ld{i}")

    def pool(T):
        bc = PL.e.alloc_register("bcD")
        PL.e.reg_mov(bc, NE * CAP - 1)
        for i in range(NT):
            PL.wait(f"d_ld{i}")
            for k in range(8):
                c = i * 8 + k
                PL.idma(f"ds{i % 3}", out=dr["xsl"], out_idx=T["desti"][:, c:c + 1],
                        in_=T[f"hb{i % 3}"][:, :], bounds_check=bc, oob_is_err=False,
                        ev=(f"d_sc{i}" if k == 7 else None))
        for i in range(NT - 3, NT):
            PL.wait(f"d_sc{i}")

    cx.phase("d", specs, {"sp": sp, "pool": pool})


def phaseM(cx, experts=None):
    S = cx.S
    dr = cx.dr
    SP, PL, ACT, DVE, PE = S["sp"], S["pool"], S["act"], S["dve"], S["pe"]
    NWB = 3
    NWD = 2
    XR = 4
    YR = 3
    specs = [(f"wg{i}", [128, 16, FF], BF16) for i in range(NWB)] + [(f"wu{i}", [128, 16, FF], BF16) for i in range(NWB)] + \
            [(f"wd{i}", [128, 4, D], BF16) for i in range(NWD)] + \
            [(f"xin{i}", [128, D], BF16) for i in range(XR)] + [(f"xT{i}", [128, 16, 128], BF16) for i in range(2)] + \
            [(f"sg{i}", [128, FF], F32) for i in range(2)] + [(f"ac{i}", [128, FF], BF16) for i in range(2)] + \
            [(f"aT{i}", [128, 4, 128], BF16) for i in range(2)] + [(f"yb{i}", [128, D], F32) for i in range(YR)] + [("scr", [128, 8], BF16)] + [
        ("TX", [128, 16, 128], BF16, "psum"), ("PG", [128, 512], F32, "psum"), ("PU", [128, 512], F32, "psum"),
        ("Y0", [128, 512], F32, "psum"), ("Y1", [128, 512], F32, "psum"), ("Y2", [128, 512], F32, "psum"), ("Y3", [128, 512], F32, "psum"),
    ]
    if experts is None:
        experts = list(range(NE + 1))
    blocks = []
    for ei, e in enumerate(experts):
        nb = JBLK if e < NE else NT
        for j in range(nb):
            if e < NE:
                src = dr["xsl"][e * CAP + j * 128:e * CAP + (j + 1) * 128, :]
                if e < NE // 2:
                    dst = dr["ysl0"][e * CAP + j * 128:e * CAP + (j + 1) * 128, :]
                else:
                    e2 = e - NE // 2
                    dst = dr["ysl1"][e2 * CAP + j * 128:e2 * CAP + (j + 1) * 128, :]
            else:
                src = dr["h2"][j * 128:(j + 1) * 128, :]
                dst = dr["ysh"][j * 128:(j + 1) * 128, :]
            blocks.append((ei, e, j, src, dst, j == nb - 1))
    NB = len(blocks)
    eblocks = {}
    for b, blk in enumerate(blocks):
        eblocks.setdefault(blk[0], []).append(b)

    def xidx(b):
        return (blocks[b][0] % 2) * 2 + (blocks[b][2] % 2)

    def wsrc(e):
        if e < NE:
            return dr["w_exp_gate"][e], dr["w_exp_up"][e], dr["w_exp_down"][e]
        return dr["w_sh_gate"], dr["w_sh_up"], dr["w_sh_down"]

    def pool(T):
        for ei, e in enumerate(experts):
            wgs, wus, wds = wsrc(e)
            if ei >= NWB:
                PL.wait(f"m_gulast{ei - NWB}")
            PL.dma(f"mw{ei % NWB}", T[f"wg{ei % NWB}"][:], wgs.rearrange("(k p) n -> p k n", p=128))
            PL.dma(f"mw{ei % NWB}", T[f"wu{ei % NWB}"][:], wus.rearrange("(k p) n -> p k n", p=128))
            S["pool"]._rec(f"m_wld{ei}", "d_" + f"mw{ei % NWB}", cx.sy.counts["d_" + f"mw{ei % NWB}"])
            if ei >= NWD:
                PL.wait(f"m_dlast{ei - NWD}")
            PL.dma(f"md{ei % NWD}", T[f"wd{ei % NWD}"][:], wds.rearrange("(c p) n -> p c n", p=128), ev=f"m_wdld{ei}")

    def make_cond(St, T):
        state = {"regs": None, "loaded": set()}

        def cond(b):
            ei, e, j = blocks[b][0], blocks[b][1], blocks[b][2]
            if e >= NE or cx.sy.dry or not DYN_SKIP:
                return None
            if state["regs"] is None:
                state["regs"] = [St.e.alloc_register(f"cnt{St.name}{p}") for p in range(2)]
            if ei not in state["loaded"]:
                state["loaded"].add(ei)
                St.e.reg_load(state["regs"][ei % 2], T["cnti"][0:1, e:e + 1])
            return (state["regs"][ei % 2], j * 128)
        return cond

    def sp(T):
        cond = make_cond(SP, T)
        sy = cx.sy

        def dma_piece(b, waits, key, out, in_, ev):
            c = cond(b)
            if sy.dry or c is None:
                SP.wait(*waits)
                SP.dma(key, out, in_, ev=ev)
                return
            w0 = dict(SP.waited)
            with SP.e.If_cmp(c[0], c[1], "IS_GT"):
                SP.wait(*waits)
                SP.dma(key, out, in_, ev=ev)
            w1 = dict(SP.waited)
            SP.waited = w0
            with SP.e.Else():
                SP.wait(*waits)
                SP.e.sem_inc(sy.sems["d_" + key], 16)
            SP.waited = w1

        def store(b):
            dma_piece(b, [f"m_yev{b}"], f"my{b % YR}", blocks[b][4], T[f"yb{b % YR}"][:], f"m_yst{b}")

        def load(b):
            ei, j = blocks[b][0], blocks[b][2]
            xi = xidx(b)
            if j >= 2:
                waits = [f"m_tx{b - 2}"]
            elif ei >= 2:
                prev = [bb for bb in eblocks[ei - 2] if blocks[bb][2] % 2 == j % 2]
                waits = [f"m_tx{prev[-1]}"]
            else:
                waits = []
            dma_piece(b, waits, f"mx{xi}", T[f"xin{xi}"][:], blocks[b][3], f"m_xld{b}")

        ne = len(experts)
        for b in eblocks[0][:2]:
            load(b)
        for ei in range(ne):
            if ei + 1 < ne:
                for b in eblocks[ei + 1][:2]:
                    load(b)
            bl = eblocks[ei]
            for j, b in enumerate(bl):
                if j + 2 < len(bl):
                    load(bl[j + 2])
                store(b)
        for b in range(max(0, NB - YR), NB):
            SP.wait(f"m_yst{b}")


    def expert_reg(St, T, regs, ei):
        e = experts[ei]
        if e >= NE or cx.sy.dry or not DYN_SKIP:
            return None
        if not regs:
            regs.extend([St.e.alloc_register(f"cn{St.name}{p}") for p in range(2)])
        St.e.reg_load(regs[ei % 2], T["cnti"][0:1, e:e + 1])
        return regs[ei % 2]

    def pe(T):
        regs = []

        def Tx(b):
            def body():
                PE.wait(f"m_xld{b}", *([f"m_xTev{b - 1}"] if b >= 1 else []))
                for k in range(16):
                    mm = PE.e.transpose(out=T["TX"][:, k, :], in_=T[f"xin{xidx(b)}"][:, k * 128:(k + 1) * 128], identity=T["identb"][:])
                PE.op(mm, ev=f"m_tx{b}")
            return body

        def GU(b):
            ei = blocks[b][0]

            def body():
                PE.wait(f"m_wld{ei}", f"m_xTev{b}", *([f"m_act{b - 1}", f"m_aTev{b - 1}", f"m_sg{b - 1}"] if b >= 1 else []))
                xT = T[f"xT{b % 2}"]
                wg, wu = T[f"wg{ei % NWB}"], T[f"wu{ei % NWB}"]
                for k in range(16):
                    PE.e.matmul(T["PG"][:, :], xT[:, k, :], wg[:, k, :], start=(k == 0), stop=(k == 15))
                    mm = PE.e.matmul(T["PU"][:, :], xT[:, k, :], wu[:, k, :], start=(k == 0), stop=(k == 15))
                PE.op(mm, ev=f"m_gu{b}")
                if blocks[b][5]:
                    S["pe"]._rec(f"m_gulast{ei}", S["pe"].semname, cx.sy.counts[S["pe"].semname])
            return body

        def TA(b):
            def body():
                PE.wait(f"m_act{b}")
                ac = T[f"ac{b % 2}"]
                pga = T["PG"][:, :].bitcast(BF16).rearrange("p (c t) -> p c t", t=128)
                for c in range(4):
                    mm = PE.e.transpose(out=pga[:, c, :], in_=ac[:, c * 128:(c + 1) * 128], identity=T["identb"][:])
                PE.op(mm, ev=f"m_ta{b}")
            return body

        def Dn(b):
            ei = blocks[b][0]

            def body():
                PE.wait(f"m_wdld{ei}", f"m_aTev{b}", *([f"m_yev{b - 1}"] if b >= 1 else []))
                aT = T[f"aT{b % 2}"]
                wd = T[f"wd{ei % NWD}"]
                for nb in range(4):
                    for c in range(4):
                        mm = PE.e.matmul(T[f"Y{nb}"][:, :], aT[:, c, :], wd[:, c, nb * 512:(nb + 1) * 512], start=(c == 0), stop=(c == 3))
                PE.op(mm, ev=f"m_d{b}")
                if blocks[b][5]:
                    S["pe"]._rec(f"m_dlast{ei}", S["pe"].semname, cx.sy.counts[S["pe"].semname])
            return body

        def tiny(k):
            PE.tick(PE.e.transpose(out=T["TX"][0:1, 0, :], in_=T["identb"][:, 0:1], identity=T["identb"][:]), k)

        PE.wait("ones_set")
        for ei in range(len(experts)):
            bl = eblocks[ei]
            pieces = [(0, 1, Tx(bl[0])), (0, 1, GU(bl[0]))]
            for j, b in enumerate(bl):
                if j + 1 < len(bl):
                    pieces.append((j + 1, 1, Tx(bl[j + 1])))
                pieces.append((j, 1, TA(b)))
                pieces.append((j, 1, Dn(b)))
                if j + 1 < len(bl):
                    pieces.append((j + 1, 1, GU(bl[j + 1])))
            PE.emit_expert(pieces, expert_reg(PE, T, regs, ei), tiny)

    def act(T):
        for b in range(NB):
            ACT.wait(f"m_gu{b}")
            if b >= 2:
                ACT.wait(f"m_act{b - 2}")
            ACT.op(ACT.e.activation(out=T[f"sg{b % 2}"][:], in_=T["PG"][:, :], func=AF.Silu), ev=f"m_sg{b}")

    def dve(T):
        regs = []

        def xTev(b):
            def body():
                DVE.wait(f"m_tx{b}", *([f"m_gu{b - 2}"] if b >= 2 else []))
                DVE.op(DVE.e.tensor_copy(out=T[f"xT{b % 2}"][:], in_=T["TX"][:]), ev=f"m_xTev{b}")
            return body

        def act_(b):
            def body():
                DVE.wait(f"m_sg{b}", *([f"m_ta{b - 2}"] if b >= 2 else []))
                DVE.op(DVE.e.tensor_tensor(out=T[f"ac{b % 2}"][:], in0=T[f"sg{b % 2}"][:], in1=T["PU"][:, :], op=ALU.mult), ev=f"m_act{b}")
            return body

        def aTev(b):
            def body():
                DVE.wait(f"m_ta{b}", *([f"m_d{b - 2}"] if b >= 2 else []))
                pga = T["PG"][:, :].bitcast(BF16).rearrange("p (c t) -> p c t", t=128)
                DVE.op(DVE.e.tensor_copy(out=T[f"aT{b % 2}"][:], in_=pga[:, 0:4, :]), ev=f"m_aTev{b}")
            return body

        def yev(b):
            def body():
                DVE.wait(f"m_d{b}", *([f"m_yst{b - YR}"] if b >= YR else []))
                for nb in range(4):
                    DVE.op(DVE.e.tensor_copy(out=T[f"yb{b % YR}"][:, nb * 512:(nb + 1) * 512], in_=T[f"Y{nb}"][:, :]), ev=(f"m_yev{b}" if nb == 3 else None))
            return body

        def tiny(k):
            DVE.tick(DVE.e.tensor_copy(out=T["scr"][:, 0:1], in_=T["zerob"][:, 0:1]), k)

        for ei in range(len(experts)):
            bl = eblocks[ei]
            pieces = [(0, 1, xTev(bl[0]))]
            for j, b in enumerate(bl):
                pieces.append((j, 1, act_(b)))
                pieces.append((j, 1, aTev(b)))
                if j + 1 < len(bl):
                    pieces.append((j + 1, 1, xTev(bl[j + 1])))
                pieces.append((j, 4, yev(b)))
            DVE.emit_expert(pieces, expert_reg(DVE, T, regs, ei), tiny)

    cx.phase("m", specs, {"pool": pool, "sp": sp, "act": act, "dve": dve, "pe": pe})


def phaseF(cx):
    S = cx.S
    dr = cx.dr
    SP, PL, ACT, DVE, PE = S["sp"], S["pool"], S["act"], S["dve"], S["pe"]
    NGK = 4
    specs = [(f"gk{i}", [128, D], F32) for i in range(NGK)] + [(f"acc{i}", [128, D], F32) for i in range(2)] + \
            [(f"x1t{i}", [128, D], F32) for i in range(2)] + [("G2b0", [128, D], F32), ("G2b1", [128, D], F32), ("junk", [128, D], BF16), ("ss", [128, 2 * NT], F32)]
    half = NE // 2 * CAP

    def pool(T):
        bc = PL.e.alloc_register("bcF")
        PL.e.reg_mov(bc, half - 1)
        n = 0
        for i in range(NT):
            for k in range(8):
                c = i * 8 + k
                if n >= NGK:
                    PL.wait(f"f_fma{n - NGK}")
                gk = T[f"gk{n % NGK}"]
                PL.idma(f"fg{n % NGK}", out=gk[:, :], in_=dr["ysl0"],
                        in_idx=T["destA"][:, c:c + 1], bounds_check=bc, oob_is_err=False)
                PL.idma(f"fg{n % NGK}", out=gk[:, :], in_=dr["ysl1"],
                        in_idx=T["destB"][:, c:c + 1], bounds_check=bc, oob_is_err=False,
                        ev=f"f_g{n}")
                n += 1

    def sp(T):
        def store(i):
            SP.wait(f"f_y{i}")
            SP.dma(f"fy{i % 2}", dr["y"][i * 128:(i + 1) * 128, :], T[f"x1t{i % 2}"][:], ev=f"f_yst{i}")
        for i in range(NT):
            s = i // 8
            if i % 8 == 0:
                if s >= 2:
                    SP.wait(f"f_y{(s - 1) * 8 - 1}")
                SP.dma(f"fG{s % 2}", T[f"G2b{s % 2}"][:], dr["modv"][3, s:s + 1, :].partition_broadcast(128), ev=f"f_G{s}")
            if i >= 2:
                SP.wait(f"f_y{i - 2}")
            SP.dma(f"fa{i % 2}", T[f"acc{i % 2}"][:], dr["ysh"][i * 128:(i + 1) * 128, :], ev=f"f_acc{i}")
            if i >= 2:
                SP.wait(f"f_yst{i - 2}")
            SP.dma(f"fx{i % 2}", T[f"x1t{i % 2}"][:], dr["x1"][i * 128:(i + 1) * 128, :], ev=f"f_x1{i}")
            if i >= 1:
                store(i - 1)
        store(NT - 1)
        SP.wait(f"f_yst{NT - 1}", f"f_yst{NT - 2}")

    def act(T):
        for i in range(NT):
            ACT.wait(f"f_sum{i}")
            ACT.op(ACT.e.activation(out=T["junk"][:], in_=T[f"acc{i % 2}"][:], func=AF.Square, accum_out=T["ss"][:, i:i + 1]), ev=f"f_ss{i}")
            ACT.wait(f"f_var{i}")
            ACT.op(ACT.e.activation(out=T["ss"][:, NT + i:NT + i + 1], in_=T["ss"][:, NT + i:NT + i + 1], func=AF.Sqrt), ev=f"f_sq{i}")

    def dve(T):
        n = 0
        ss = T["ss"]
        for i in range(NT):
            acc = T[f"acc{i % 2}"]
            DVE.wait(f"f_acc{i}")
            for k in range(8):
                c = i * 8 + k
                DVE.wait(f"f_g{n}")
                DVE.op(DVE.e.scalar_tensor_tensor(out=acc[:], in0=T[f"gk{n % NGK}"][:], scalar=T["gatek"][:, c:c + 1], in1=acc[:], op0=ALU.mult, op1=ALU.add),
                       ev=f"f_fma{n}")
                DVE.dep()
                n += 1
            S["dve"]._rec(f"f_sum{i}", S["dve"].semname, cx.sy.counts[S["dve"].semname])
            DVE.wait(f"f_ss{i}")
            DVE.op(DVE.e.tensor_scalar(out=ss[:, NT + i:NT + i + 1], in0=ss[:, i:i + 1], scalar1=1.0 / D, scalar2=EPS, op0=ALU.mult, op1=ALU.add), ev=f"f_var{i}")
            DVE.wait(f"f_sq{i}")
            DVE.op(DVE.e.reciprocal(out=ss[:, NT + i:NT + i + 1], in_=ss[:, NT + i:NT + i + 1]))
            DVE.dep()
            DVE.wait(f"f_G{i // 8}", f"f_x1{i}")
            DVE.op(DVE.e.scalar_tensor_tensor(out=acc[:], in0=acc[:], scalar=ss[:, NT + i:NT + i + 1], in1=T[f"G2b{(i // 8) % 2}"][:], op0=ALU.mult, op1=ALU.mult))
            DVE.dep()
            DVE.op(DVE.e.tensor_tensor(out=T[f"x1t{i % 2}"][:], in0=T[f"x1t{i % 2}"][:], in1=acc[:], op=ALU.add), ev=f"f_y{i}")

    cx.phase("f", specs, {"pool": pool, "sp": sp, "act": act, "dve": dve})


_CACHE = {}


def kernel(**inputs):
    inputs = {k: np.asarray(v) for k, v in inputs.items()}
    if "nc" not in _CACHE:
        _CACHE["nc"] = build_program()
    nc = _CACHE["nc"]
    in_maps = [make_inputs_for_core(c, inputs) for c in range(NCORES)]
    res = run_bass_kernel_spmd(nc, in_maps, core_ids=list(range(NCORES)))
    ys = [np.asarray(r["y"]) for r in res.results]
    y_prompt = np.empty((2, 8192, D), np.float32)
    y_sample = np.empty((1, 8192, D), np.float32)
    for c in range(NCORES):
        lo = c * SEGT
        y_prompt[0, lo:lo + SEGT] = ys[c][0:SEGT]
        y_prompt[1, lo:lo + SEGT] = ys[c][SEGT:2 * SEGT]
        y_sample[0, lo:lo + SEGT] = ys[c][2 * SEGT:3 * SEGT]
    return (y_prompt, y_sample)
```

```python
from contextlib import ExitStack
import numpy as np
import concourse.bass as bass
import concourse.mybir as mybir
from concourse.bass_utils import run_bass_kernel_spmd

F32 = mybir.dt.float32
BF16 = mybir.dt.bfloat16
I32 = mybir.dt.int32
U32 = mybir.dt.uint32
AF = mybir.ActivationFunctionType
ALU = mybir.AluOpType
AX = mybir.AxisListType

NCORES = 8
D = 2048
NSEG = 3
SEGT = 1024
NTOK = NSEG * SEGT
NT = NTOK // 128
GRP = 512
NG = NTOK // GRP
NE = 64
FF = 512
DYN_SKIP = True
JBLK = 8
CAP = JBLK * 128
EPS = 1e-6
BIG = 1.0e4


class _Dummy:
    def __getattr__(self, k):
        return lambda *a, **kw: _Dummy()

    def __getitem__(self, k):
        return _Dummy()


class Sync:
    def __init__(self, nc, stack):
        self.nc = nc
        self.stack = stack
        self.ev = {}
        self.sems = {}
        self.counts = {}
        self.dry = True

    def sem(self, name):
        if name not in self.sems:
            self.sems[name] = self.stack.enter_context(self.nc.semaphore(name))
            self.counts[name] = 0
        return self.sems[name]

    def reset_pass(self, dry):
        self.dry = dry
        for k in self.counts:
            self.counts[k] = 0


class Stream:
    def __init__(self, sy, name):
        self.sy = sy
        self.name = name
        self.semname = "s_" + name
        sy.sem(self.semname)
        self.eng = _Dummy()
        self.waited = {}

    def begin(self, eng):
        self.eng = _Dummy() if self.sy.dry else eng
        self.waited = {}

    @property
    def e(self):
        return self.eng

    def _rec(self, ev, sn, n):
        sy = self.sy
        if ev is None:
            return
        if sy.dry:
            assert ev not in sy.ev, ev
            sy.ev[ev] = (sn, n)
        else:
            assert sy.ev[ev] == (sn, n), (ev, sy.ev[ev], sn, n)

    def op(self, instr, ev=None):
        sy = self.sy
        sy.counts[self.semname] += 1
        n = sy.counts[self.semname]
        if not sy.dry:
            instr.then_inc(sy.sems[self.semname], 1)
        self._rec(ev, self.semname, n)
        return n

    def dep(self):
        if not self.sy.dry:
            n = self.sy.counts[self.semname]
            if n > 0 and self.waited.get(self.semname, 0) < n:
                self.eng.wait_ge(self.sy.sems[self.semname], n)
                self.waited[self.semname] = n

    def dma(self, key, out, in_, ev=None, **kw):
        sy = self.sy
        sn = "d_" + key
        sy.sem(sn)
        sy.counts[sn] += 16
        n = sy.counts[sn]
        if not sy.dry:
            self.eng.dma_start(out=out, in_=in_, **kw).then_inc(sy.sems[sn], 16)
        self._rec(ev, sn, n)
        return n

    def idma(self, key, ev=None, **kw):
        sy = self.sy
        sn = "d_" + key
        sy.sem(sn)
        sy.counts[sn] += 16
        n = sy.counts[sn]
        if not sy.dry:
            oi = kw.pop("out_idx", None)
            ii = kw.pop("in_idx", None)
            kw["out_offset"] = bass.IndirectOffsetOnAxis(ap=oi, axis=0) if oi is not None else None
            kw["in_offset"] = bass.IndirectOffsetOnAxis(ap=ii, axis=0) if ii is not None else None
            self.eng.indirect_dma_start(**kw).then_inc(sy.sems[sn], 16)
        self._rec(ev, sn, n)
        return n

    def piece(self, cond, body, skip):
        sy = self.sy
        if sy.dry or cond is None:
            body()
            return
        n0 = sy.counts[self.semname]
        w0 = dict(self.waited)
        with self.eng.If_cmp(cond[0], cond[1], "IS_GT"):
            body()
        k = sy.counts[self.semname] - n0
        w1 = dict(self.waited)
        self.waited = w0
        with self.eng.Else():
            skip(k)
        self.waited = {kk: min(w1.get(kk, 0), self.waited.get(kk, 0)) for kk in set(w1) | set(self.waited)}

    def emit_expert(self, pieces, reg, tiny, always=2):
        sy = self.sy
        if sy.dry or reg is None:
            for (j, k, body) in pieces:
                body()
            return
        sn = self.semname

        def rec(i, lo):
            while i < len(pieces) and pieces[i][0] <= lo:
                pieces[i][2]()
                i += 1
            if i == len(pieces):
                return
            n0 = sy.counts[sn]
            w0 = dict(self.waited)
            n_end = n0 + sum(k for (j, k, body) in pieces[i:])
            with self.eng.If_cmp(reg, (lo + 1) * 128, "IS_LE"):
                pend = 0
                for (j, k, body) in pieces[i:]:
                    if j <= lo:
                        if pend:
                            tiny(pend)
                            sy.counts[sn] += pend
                            pend = 0
                        body()
                    else:
                        pend += k
                if pend:
                    tiny(pend)
                    sy.counts[sn] += pend
            assert sy.counts[sn] == n_end, (sy.counts[sn], n_end)
            w_a = dict(self.waited)
            sy.counts[sn] = n0
            self.waited = w0
            with self.eng.Else():
                pieces[i][2]()
                rec(i + 1, lo + 1)
            assert sy.counts[sn] == n_end, (sy.counts[sn], n_end)
            self.waited = {kk: min(w_a.get(kk, 0), self.waited.get(kk, 0)) for kk in set(w_a) | set(self.waited)}
        rec(0, always - 1)

    def tick(self, instr, k):
        if not self.sy.dry and k > 0:
            instr.then_inc(self.sy.sems[self.semname], k)

    def wait(self, *evs):
        if self.sy.dry:
            return
        for ev in evs:
            if ev is None:
                continue
            sn, n = self.sy.ev[ev]
            if self.waited.get(sn, 0) >= n:
                continue
            self.eng.wait_ge(self.sy.sems[sn], n)
            self.waited[sn] = n


class Ctx:
    def __init__(self, nc, sy, dram):
        self.nc = nc
        self.sy = sy
        self.dr = dram
        self.S = {n: Stream(sy, n) for n in ("pe", "act", "dve", "pool", "sp")}
        self.P = {}

    def alloc(self, st, specs):
        T = {}
        for name, shape, dt, *kind in specs:
            if self.sy.dry:
                T[name] = _Dummy()
            elif kind and kind[0] == "psum":
                T[name] = st.enter_context(self.nc.psum_tensor(f"t{int(self.sy.dry)}_" + name, list(shape), dt))
            else:
                T[name] = st.enter_context(self.nc.sbuf_tensor(f"t{int(self.sy.dry)}_" + name, list(shape), dt))
        return T

    def phase(self, name, specs, progs):
        S = self.S
        self.uid = getattr(self, "uid", 0) + 1
        name = f"{name}u{self.uid}"
        with ExitStack() as st:
            T = self.alloc(st, [(name + "_" + s[0],) + tuple(s[1:]) for s in specs])
            T = {k[len(name) + 1:]: v for k, v in T.items()}
            T.update(self.P)
            if self.sy.dry:
                for en in ("sp", "pool", "act", "dve", "pe"):
                    if en in progs:
                        S[en].begin(None)
                        progs[en](T)
            else:
                with self.nc.Block() as blk:
                    reg = {"sp": blk.sync, "pool": blk.gpsimd, "act": blk.scalar, "dve": blk.vector, "pe": blk.tensor}
                    for en in ("sp", "pool", "act", "dve", "pe"):
                        if en in progs:
                            def f(eng, en=en):
                                S[en].begin(eng)
                                progs[en](T)
                            reg[en](f)


def bcast3(ap2d, a, b):
    return ap2d.unsqueeze(2).to_broadcast([128, a, b])


def phase0(cx):
    S = cx.S
    dr = cx.dr
    SP, PL, ACT, DVE, PE = S["sp"], S["pool"], S["act"], S["dve"], S["pe"]
    NB = 48
    specs = [
        ("cT", [128, 3, 16], F32), ("wa0", [128, 16, 256], F32), ("wa1", [128, 16, 256], F32), ("wa2", [128, 16, 256], F32),
        ("mod", [3, 6 * D], F32), ("der", [3, 4, D], F32),
        ("wsf", [128, 8, 128], F32),
        ("pm0", [128, 512], F32, "psum"), ("pm1", [128, 512], F32, "psum"),
        ("pws", [128, 8, 128], F32, "psum"),
    ]

    def sp(T):
        for b in range(3):
            SP.dma("c", T["cT"][:, b, :], dr["cvec"][b:b + 1, :].rearrange("o (k p) -> p (o k)", p=128), allow_slow_non_contiguous=True)
        SP.dma("c", T["mod"][:], dr["b_ada"].partition_broadcast(3))
        for i, nm in enumerate(("g_pre_mix", "g_post_mix", "g_pre_ffn", "g_post_ffn")):
            SP.dma("c", T["der"][:, i, :], dr[nm].partition_broadcast(3), ev=("c_all" if i == 3 else None))
        for b in range(NB):
            if b >= 3:
                SP.wait(f"mod_pe{b - 3}")
            SP.dma(f"wa{b % 3}", T[f"wa{b % 3}"][:], dr["w_ada"][:, b * 256:(b + 1) * 256].rearrange("(k p) n -> p k n", p=128), ev=f"wa_ld{b}")
        SP.dma("k", T["identf"][:], dr["ident"])
        SP.dma("k", T["wsf"][:], dr["w_spatial"].rearrange("h p q -> p h q"))
        SP.dma("k", T["bsb"][:], dr["b_spatial"].partition_broadcast(128))
        SP.dma("k", T["lngb"][:], dr["ln_v_g"].partition_broadcast(128))
        SP.dma("k", T["lnbb"][:], dr["ln_v_b"].partition_broadcast(128))
        for t in range(3):
            SP.dma("k", T["convw"][:, t, :], dr["conv_w"][t:t + 1, :].rearrange("o (c p) -> p (o c)", p=128), allow_slow_non_contiguous=True)
        SP.dma("k", T["goa"][:], dr["g_out_a"].rearrange("o (c p) -> p (o c)", p=128), allow_slow_non_contiguous=True)
        SP.dma("k", T["gob"][:], dr["g_out_b"].rearrange("o (c p) -> p (o c)", p=128), allow_slow_non_contiguous=True)
        SP.dma("k", T["wr"][:], dr["w_router"].rearrange("(k p) e -> p k e", p=128))
        SP.dma("k", T["rbias"][:], dr["router_bias"].partition_broadcast(128))
        SP.dma("k", T["ecap"][:], dr["ecap"].partition_broadcast(128))
        SP.dma("k", T["hmask"][:], dr["hmask"], ev="k_all")
        SP.wait("der_done")
        for i in range(4):
            SP.dma("mv", dr["modv"][i], T["der"][:, i, :])
        SP.dma("mv", dr["modv"][4], T["mod"][:, 0:D])
        SP.dma("mv", dr["modv"][5], T["mod"][:, 3 * D:4 * D], ev="modv_st")
        SP.wait("modv_st")

    def pool(T):
        PL.dma("kb", T["identb"][:], dr["ident"])
        PL.dma("kb", T["trib"][:], dr["tri"], ev="kb_all")
        PL.op(PL.e.memset(T["zerob"][:], 0.0))
        PL.op(PL.e.memset(T["onesb"][:], 1.0), ev="ones_set")
        PL.wait("kb_all")

    def act(T):
        ACT.wait("c_all")
        ACT.op(ACT.e.activation(out=T["cT"][:], in_=T["cT"][:], func=AF.Silu), ev="silu_c")
        ACT.wait("wsT_pe")
        ACT.op(ACT.e.copy(out=T["wsT"][:], in_=T["pws"][:]), ev="wsT_done")

    def dve(T):
        DVE.wait("c_all")
        mod = T["mod"]
        for b in range(NB):
            DVE.wait(f"mod_pe{b}")
            pm = T[f"pm{b % 2}"]
            DVE.op(DVE.e.tensor_tensor(out=mod[:, b * 256:(b + 1) * 256], in0=pm[0:3, 0:256], in1=mod[:, b * 256:(b + 1) * 256], op=ALU.add), ev=f"mod_ev{b}")
        DVE.dep()
        DVE.op(DVE.e.scalar_tensor_tensor(out=T["der"][:, 0, :], in0=mod[:, D:2 * D], scalar=1.0, in1=T["der"][:, 0, :], op0=ALU.add, op1=ALU.mult))
        DVE.op(DVE.e.tensor_tensor(out=T["der"][:, 1, :], in0=mod[:, 2 * D:3 * D], in1=T["der"][:, 1, :], op=ALU.mult))
        DVE.op(DVE.e.scalar_tensor_tensor(out=T["der"][:, 2, :], in0=mod[:, 4 * D:5 * D], scalar=1.0, in1=T["der"][:, 2, :], op0=ALU.add, op1=ALU.mult))
        DVE.op(DVE.e.tensor_tensor(out=T["der"][:, 3, :], in0=mod[:, 5 * D:6 * D], in1=T["der"][:, 3, :], op=ALU.mult), ev="der_done")

    def pe(T):
        PE.wait("silu_c")
        for b in range(NB):
            PE.wait(f"wa_ld{b}")
            if b >= 2:
                PE.wait(f"mod_ev{b - 2}")
            pm = T[f"pm{b % 2}"]
            wa = T[f"wa{b % 3}"]
            for k in range(16):
                mm = PE.e.matmul(pm[0:3, 0:256], T["cT"][:, :, k], wa[:, k, :], start=(k == 0), stop=(k == 15))
            PE.op(mm, ev=f"mod_pe{b}")
        PE.wait("k_all")
        for h in range(8):
            mm = PE.e.transpose(out=T["pws"][:, h, :], in_=T["wsf"][:, h, :], identity=T["identf"][:])
        PE.op(mm, ev="wsT_pe")

    cx.phase("p0", specs, {"sp": sp, "pool": pool, "act": act, "dve": dve, "pe": pe})


def phase0b(cx):
    S = cx.S
    dr = cx.dr
    SP, PL, ACT, DVE, PE = S["sp"], S["pool"], S["act"], S["dve"], S["pe"]
    specs = [
        ("xh", [12, D], F32), ("a1h", [12, D], F32), ("b1h", [12, D], F32), ("hjunk", [12, D], BF16),
        ("htmp", [12, D], F32), ("hh", [12, D], BF16), ("hss", [12, 2], F32),
        ("pth", [128, 16, 12], BF16, "psum"),
    ]

    def sp(T):
        SP.dma("hl", T["xh"][:], dr["xhalo"])
        for s in range(NSEG):
            SP.dma("hl", T["a1h"][4 * s:4 * s + 4, :], dr["modv"][0, s:s + 1, :].partition_broadcast(4))
            SP.dma("hl", T["b1h"][4 * s:4 * s + 4, :], dr["modv"][4, s:s + 1, :].partition_broadcast(4), ev=("hl_all" if s == NSEG - 1 else None))

    def act(T):
        ACT.wait("hl_all")
        ACT.op(ACT.e.activation(out=T["hjunk"][:], in_=T["xh"][:], func=AF.Square, accum_out=T["hss"][:, 0:1]), ev="h_ssq")
        ACT.wait("h_var")
        ACT.op(ACT.e.activation(out=T["hss"][:, 1:2], in_=T["hss"][:, 1:2], func=AF.Sqrt), ev="h_sqrt")

    def dve(T):
        DVE.wait("h_ssq")
        DVE.op(DVE.e.tensor_scalar(out=T["hss"][:, 1:2], in0=T["hss"][:, 0:1], scalar1=1.0 / D, scalar2=EPS, op0=ALU.mult, op1=ALU.add), ev="h_var")
        DVE.wait("h_sqrt")
        DVE.op(DVE.e.reciprocal(out=T["hss"][:, 1:2], in_=T["hss"][:, 1:2]))
        DVE.dep()
        DVE.op(DVE.e.scalar_tensor_tensor(out=T["htmp"][:], in0=T["xh"][:], scalar=T["hss"][:, 1:2], in1=T["a1h"][:], op0=ALU.mult, op1=ALU.mult))
        DVE.dep()
        DVE.op(DVE.e.tensor_tensor(out=T["hh"][:], in0=T["htmp"][:], in1=T["b1h"][:], op=ALU.add), ev="hh_done")
        DVE.wait("hT_pe")
        DVE.op(DVE.e.tensor_copy(out=T["hTh"][:], in_=T["pth"][:]), ev="hTh_done")

    def pe(T):
        PE.wait("hh_done")
        for k in range(16):
            mm = PE.e.transpose(out=T["pth"][:, k, :], in_=T["hh"][:, k * 128:(k + 1) * 128], identity=T["identb"][0:12, 0:12])
        PE.op(mm, ev="hT_pe")

    cx.phase("p0b", specs, {"sp": sp, "act": act, "dve": dve, "pe": pe})


def phaseP1(cx, g):
    S = cx.S
    dr = cx.dr
    SP, PL, ACT, DVE, PE = S["sp"], S["pool"], S["act"], S["dve"], S["pe"]
    s = g // 2
    specs = [(f"xt{i}", [128, D], F32) for i in range(4)] + [
        ("junk", [128, D], BF16), ("tmpf", [128, D], F32), ("htok0", [128, D], BF16), ("htok1", [128, D], BF16),
        ("A1b", [128, D], F32), ("B1b", [128, D], F32), ("ss", [128, 8], F32),
        ("pt0", [128, 16, 128], BF16, "psum"), ("pt1", [128, 16, 128], BF16, "psum"),
    ]
    G = f"{g}"

    def sp(T):
        SP.dma("ab", T["A1b"][:], dr["modv"][0, s:s + 1, :].partition_broadcast(128))
        SP.dma("ab", T["B1b"][:], dr["modv"][4, s:s + 1, :].partition_broadcast(128), ev="ab_ld" + G)
        for i in range(4):
            r0 = g * GRP + i * 128
            SP.dma(f"xt{i}", T[f"xt{i}"][:], dr["xs"][r0:r0 + 128, :], ev=f"xt_ld{G}_{i}")

    def act(T):
        for i in range(4):
            ACT.wait(f"xt_ld{G}_{i}")
            ACT.op(ACT.e.activation(out=T["junk"][:], in_=T[f"xt{i}"][:], func=AF.Square, accum_out=T["ss"][:, i:i + 1]), ev=f"ssq{G}_{i}")
        ACT.wait("var" + G)
        ACT.op(ACT.e.activation(out=T["ss"][:, 4:8], in_=T["ss"][:, 4:8], func=AF.Sqrt), ev="sqrt" + G)
        for i in range(4):
            ACT.wait(f"tp{G}_{i}")
            ACT.op(ACT.e.copy(out=T["hT"][:, 0:8, i * 128:(i + 1) * 128], in_=T[f"pt{i % 2}"][:, 0:8, :]), ev=f"hTa{G}_{i}")

    def dve(T):
        DVE.wait(f"ssq{G}_3")
        DVE.op(DVE.e.tensor_scalar(out=T["ss"][:, 4:8], in0=T["ss"][:, 0:4], scalar1=1.0 / D, scalar2=EPS, op0=ALU.mult, op1=ALU.add), ev="var" + G)
        DVE.wait("sqrt" + G)
        DVE.op(DVE.e.reciprocal(out=T["ss"][:, 4:8], in_=T["ss"][:, 4:8]))
        DVE.dep()
        DVE.wait("ab_ld" + G)

        def evac(i):
            DVE.wait(f"tp{G}_{i}")
            DVE.op(DVE.e.tensor_copy(out=T["hT"][:, 8:16, i * 128:(i + 1) * 128], in_=T[f"pt{i % 2}"][:, 8:16, :]), ev=f"hTb{G}_{i}")
        for i in range(4):
            DVE.op(DVE.e.scalar_tensor_tensor(out=T["tmpf"][:], in0=T[f"xt{i}"][:], scalar=T["ss"][:, 4 + i:5 + i], in1=T["A1b"][:], op0=ALU.mult, op1=ALU.mult))
            DVE.dep()
            if i >= 2:
                DVE.wait(f"tp{G}_{i - 2}")
            DVE.op(DVE.e.tensor_tensor(out=T[f"htok{i % 2}"][:], in0=T["tmpf"][:], in1=T["B1b"][:], op=ALU.add), ev=f"htok{G}_{i}")
            if i >= 1:
                evac(i - 1)
        evac(3)

    def pe(T):
        PE.wait("kb_all")
        for i in range(4):
            PE.wait(f"htok{G}_{i}")
            if i >= 2:
                PE.wait(f"hTa{G}_{i - 2}", f"hTb{G}_{i - 2}")
            for k in range(16):
                mm = PE.e.transpose(out=T[f"pt{i % 2}"][:, k, :], in_=T[f"htok{i % 2}"][:, k * 128:(k + 1) * 128], identity=T["identb"][:])
            PE.op(mm, ev=f"tp{G}_{i}")

    cx.phase("p1", specs, {"sp": sp, "act": act, "dve": dve, "pe": pe})


IN_SPECS = [
    ("xs", [NTOK, D], F32), ("xhalo", [12, D], F32), ("hmask", [128, 12], F32), ("cvec", [3, D], F32),
    ("w_ada", [D, 6 * D], F32), ("b_ada", [1, 6 * D], F32), ("g_pre_mix", [1, D], F32), ("w_in", [D, 5120], F32),
    ("conv_w", [3, 1024], F32), ("ln_v_g", [1, 1024], F32), ("ln_v_b", [1, 1024], F32), ("w_spatial", [8, 128, 128], F32),
    ("b_spatial", [1, 1024], F32), ("g_out_a", [1, 1024], F32), ("g_out_b", [1, 1024], F32), ("w_out", [D, D], F32),
    ("g_post_mix", [1, D], F32), ("g_pre_ffn", [1, D], F32), ("w_router", [D, NE], F32), ("router_bias", [1, NE], F32),
    ("w_exp_gate", [NE, D, FF], F32), ("w_exp_up", [NE, D, FF], F32), ("w_exp_down", [NE, FF, D], F32),
    ("w_sh_gate", [D, FF], F32), ("w_sh_up", [D, FF], F32), ("w_sh_down", [FF, D], F32), ("g_post_ffn", [1, D], F32),
    ("ident", [128, 128], F32), ("tri", [128, 128], F32), ("ecap", [1, NE], F32),
]

PERSIST_G = [
    ("identf", [128, 128], F32), ("identb", [128, 128], BF16), ("trib", [128, 128], BF16), ("onesb", [128, 128], BF16), ("zerob", [128, 16], BF16),
    ("rbias", [128, NE], F32), ("ecap", [128, NE], F32),
    ("LG", [128, NT, NE], F32), ("desti", [128, NT * 8], I32), ("destA", [128, NT * 8], I32), ("destB", [128, NT * 8], I32),
    ("gatek", [128, NT * 8], F32), ("cnti", [128, NE], I32),
]
PERSIST_A = [
    ("wsT", [128, 8, 128], BF16), ("bsb", [128, 1024], F32), ("lngb", [128, 1024], F32), ("lnbb", [128, 1024], F32),
    ("convw", [128, 3, 8], F32), ("goa", [128, 8], F32), ("gob", [128, 8], F32), ("wr", [128, 16, NE], F32),
    ("hmask", [128, 12], F32), ("hTh", [128, 16, 12], BF16), ("zhalo", [128, 8, 12], F32),
    ("hT", [128, 16, GRP], BF16), ("mixin", [128, 16, GRP], BF16), ("rstdab", [128, 8], F32),
]


def build_program(debug=None):
    nc = bass.Bass("TRN2", target_bir_lowering=False)
    dr = {}
    for name, shape, dt in IN_SPECS:
        if debug in ("p1", "p2", "q", "r") and name.startswith("w_exp"):
            continue
        dr[name] = nc.dram_tensor(name, shape, dt, kind="ExternalInput").ap()
    dr["y"] = nc.dram_tensor("y", [NTOK, D], F32, kind="ExternalOutput").ap()
    dr["modv"] = nc.dram_tensor("modv", [6, 3, D], F32, kind="Internal").ap()
    dr["x1"] = nc.dram_tensor("x1s", [NTOK, D], F32, kind="Internal").ap()
    dr["h2"] = nc.dram_tensor("h2s", [NTOK, D], BF16, kind="Internal").ap()
    dr["xsl"] = nc.dram_tensor("xsl", [NE * CAP, D], BF16, kind="Internal").ap()
    dr["ysl0"] = nc.dram_tensor("ysl0", [NE // 2 * CAP, D], F32, kind="Internal").ap()
    dr["ysl1"] = nc.dram_tensor("ysl1", [NE // 2 * CAP, D], F32, kind="Internal").ap()
    dr["ysh"] = nc.dram_tensor("ysh", [NTOK, D], F32, kind="Internal").ap()
    if debug == "r":
        dr["dbg_desti"] = nc.dram_tensor("dbg_desti", [128, NT * 8], I32, kind="ExternalOutput").ap()
        dr["dbg_destA"] = nc.dram_tensor("dbg_destA", [128, NT * 8], I32, kind="ExternalOutput").ap()
        dr["dbg_destB"] = nc.dram_tensor("dbg_destB", [128, NT * 8], I32, kind="ExternalOutput").ap()
        dr["dbg_gatek"] = nc.dram_tensor("dbg_gatek", [128, NT * 8], F32, kind="ExternalOutput").ap()
        dr["dbg_LG"] = nc.dram_tensor("dbg_LG", [128, NT, NE], F32, kind="ExternalOutput").ap()
    elif debug:
        dr["dbg_hT"] = nc.dram_tensor("dbg_hT", [128, 16, GRP], F32, kind="ExternalOutput").ap()
        dr["dbg_mod"] = nc.dram_tensor("dbg_mod", [6, 3, D], F32, kind="ExternalOutput").ap()
        dr["dbg_hTh"] = nc.dram_tensor("dbg_hTh", [128, 16, 12], F32, kind="ExternalOutput").ap()
        dr["dbg_mixin"] = nc.dram_tensor("dbg_mixin", [128, 16, GRP], F32, kind="ExternalOutput").ap()
        dr["dbg_rstdab"] = nc.dram_tensor("dbg_rstdab", [128, 8], F32, kind="ExternalOutput").ap()
        dr["dbg_zhalo"] = nc.dram_tensor("dbg_zhalo", [128, 8, 12], F32, kind="ExternalOutput").ap()
        dr["dbg_LG"] = nc.dram_tensor("dbg_LG", [128, NT, NE], F32, kind="ExternalOutput").ap()
        dr["dbg_x1"] = nc.dram_tensor("dbg_x1", [1024, D], F32, kind="ExternalOutput").ap()
        dr["dbg_h2"] = nc.dram_tensor("dbg_h2", [1024, D], BF16, kind="ExternalOutput").ap()
    with ExitStack() as stack:
        sy = Sync(nc, stack)
        cx = Ctx(nc, sy, dr)
        for real in (False, True):
            sy.reset_pass(dry=not real)
            cx.uid = 0
            with ExitStack() as pst:
                cx.P = cx.alloc(pst, PERSIST_G)
                emit_all(cx, debug)
    return nc


def phase_debug_dump(cx, what):
    S = cx.S
    dr = cx.dr
    SP, DVE = S["sp"], S["dve"]
    specs = [("f", [128, 16, GRP], F32), ("f2", [128, 16, 12], F32), ("f3", [128, 16, GRP], F32)]

    def dve(T):
        DVE.op(DVE.e.tensor_copy(out=T["f"][:], in_=T["hT"][:]))
        DVE.op(DVE.e.tensor_copy(out=T["f3"][:], in_=T["mixin"][:]))
        DVE.op(DVE.e.tensor_copy(out=T["f2"][:], in_=T["hTh"][:]), ev="dbg_cp")

    def sp(T):
        SP.wait("dbg_cp")
        SP.dma("dbg", dr["dbg_hT"], T["f"][:])
        SP.dma("dbg", dr["dbg_mixin"], T["f3"][:])
        SP.dma("dbg", dr["dbg_hTh"], T["f2"][:])
        SP.dma("dbg", dr["dbg_rstdab"], T["rstdab"][:])
        SP.dma("dbg", dr["dbg_zhalo"], T["zhalo"][:])
        SP.dma("dbg", dr["dbg_LG"], T["LG"][:])
        SP.dma("dbg", dr["dbg_x1"], dr["x1"][0:1024, :])
        SP.dma("dbg", dr["dbg_h2"], dr["h2"][0:1024, :])
        SP.dma("dbg", dr["dbg_mod"], dr["modv"], ev="dbg_st")
        SP.wait("dbg_st")
    cx.phase("dbg", specs, {"sp": sp, "dve": dve})


def emit_all(cx, debug):
    PG = dict(cx.P)
    with ExitStack() as ast:
        cx.P = dict(PG)
        cx.P.update(cx.alloc(ast, PERSIST_A))
        phase0(cx)
        phase0b(cx)
        ngroups = 2 if debug in ("p1", "p2", "q") else NG
        for g in range(ngroups):
            phaseP1(cx, g)
            if debug in ("p1",):
                continue
            phaseP2(cx, g)
            if debug in ("p2",):
                continue
            phaseQ(cx, g)
        if debug in ("p1", "p2", "q"):
            phase_debug_dump(cx, debug)
            return
    cx.P = dict(PG)
    phaseR(cx)
    phaseD(cx)
    if debug == "r":
        phase_debug_r(cx)
        return
    phaseM(cx)
    phaseF(cx)


def phase_debug_r(cx):
    S = cx.S
    dr = cx.dr
    SP = S["sp"]

    def sp(T):
        SP.dma("dbg", dr["dbg_desti"], T["desti"][:])
        SP.dma("dbg", dr["dbg_destA"], T["destA"][:])
        SP.dma("dbg", dr["dbg_destB"], T["destB"][:])
        SP.dma("dbg", dr["dbg_gatek"], T["gatek"][:])
        SP.dma("dbg", dr["dbg_LG"], T["LG"][:], ev="dbgr_st")
        SP.wait("dbgr_st")
    cx.phase("dbgr", [], {"sp": sp})


def make_inputs_for_core(c, inputs, skip=()):
    xp, xsm = inputs["x_prompt"], inputs["x_sample"]
    seqs = [xp[0], xp[1], xsm[0]]
    lo = c * SEGT
    xs = np.concatenate([sq[lo:lo + SEGT] for sq in seqs], axis=0)
    xhalo = np.zeros((12, D), np.float32)
    hmask = np.zeros((128, 12), np.float32)
    for g in range(NG):
        s, half = g // 2, g % 2
        st = lo + half * GRP
        if st - 1 >= 0:
            xhalo[2 * g] = seqs[s][st - 1]
            hmask[:, 2 * g] = 1.0
        if st + GRP < seqs[s].shape[0]:
            xhalo[2 * g + 1] = seqs[s][st + GRP]
            hmask[:, 2 * g + 1] = 1.0
    cvec = np.stack([inputs["c_prompt"][0], inputs["c_prompt"][1], inputs["c_sample"][0]], axis=0)
    m = {"xs": np.ascontiguousarray(xs), "xhalo": xhalo, "hmask": hmask, "cvec": np.ascontiguousarray(cvec)}
    for name, shape, dt in IN_SPECS:
        if name in m or name in ("ident", "tri", "ecap") or name in skip:
            continue
        m[name] = np.ascontiguousarray(np.asarray(inputs[name])[0]).reshape(shape)
    m["ident"] = np.eye(128, dtype=np.float32)
    m["tri"] = np.triu(np.ones((128, 128), np.float32), k=1)
    m["ecap"] = (np.arange(NE, dtype=np.float32) * CAP).reshape(1, NE)
    return m


def phaseP2(cx, g):
    S = cx.S
    dr = cx.dr
    SP, PL, ACT, DVE, PE = S["sp"], S["pool"], S["act"], S["dve"], S["pe"]
    G = f"{g}"
    NWB = 4
    specs = [(f"w{i}", [128, 16, 256], BF16) for i in range(NWB)] + [
        ("t1", [128, GRP], F32), ("z", [128, GRP + 2], F32), ("c0", [128, GRP], F32), ("c1", [128, GRP], F32),
        ("raw0", [128, GRP], F32), ("raw1", [128, GRP], F32), ("sq0", [128, GRP], BF16), ("sq1", [128, GRP], BF16),
        ("ug", [128, 8, GRP], BF16),
        ("vg0", [128, 1024], F32), ("vg1", [128, 1024], F32), ("vg2", [128, 1024], F32), ("vg3", [128, 1024], F32),
        ("vsq0", [128, 1024], F32), ("vsq1", [128, 1024], F32), ("vsq2", [128, 1024], F32), ("vsq3", [128, 1024], F32), ("vn", [128, 1024], F32),
        ("vnb0", [128, 1024], BF16), ("vnb1", [128, 1024], BF16), ("vnb2", [128, 1024], BF16), ("vnb3", [128, 1024], BF16),
        ("vs", [128, 4, 32], F32), ("spt", [128, GRP], F32), ("zh", [128, 12], F32), ("stt", [128, 8], F32),
        ("pA", [128, 512], F32, "psum"), ("pB", [128, 512], F32, "psum"), ("pC", [128, 512], F32, "psum"),
        ("pD", [128, 512], F32, "psum"), ("pE", [128, 512], F32, "psum"), ("pF", [128, 512], F32, "psum"),
        ("pst", [128, 512], F32, "psum"), ("ph", [128, 2, 12], F32, "psum"),
    ]
    blocks = []
    for vj in range(4):
        blocks.append(("v", vj, 4096 + vj * 256))
    for cj in range(4):
        blocks += [("cg", cj, 1024 + cj * 256), ("xh", cj, 2048 + cj * 256), ("bg", cj, cj * 256)]
    for uj in range(4):
        blocks.append(("u", uj, 3072 + uj * 256))
    bidx = {(n, j): i for i, (n, j, c) in enumerate(blocks)}

    def wld(n, j):
        return f"w_ld{G}_{bidx[(n, j)]}"

    def wbuf(T, n, j):
        return T[f"w{bidx[(n, j)] % NWB}"]

    def pool(T):
        for i, (n, j, c0) in enumerate(blocks):
            if i >= NWB:
                PL.wait(f"w_free{G}_{i - NWB}")
            PL.dma(f"w{i % NWB}", T[f"w{i % NWB}"][:], dr["w_in"][:, c0:c0 + 256].rearrange("(k p) n -> p k n", p=128), ev=f"w_ld{G}_{i}")

    cbanks = [("pA", "pB", "pC"), ("pD", "pE", "pF")]

    def pe(T):
        hT = T["hT"]
        def stats(c, br):
            PE.wait(f"sq{br}{G}_{c}")
            sq = T[f"sq{c % 2}"]
            for i in range(4):
                col = (0 if br == "a" else 4) + i
                mm = PE.e.matmul(T["pst"][:, col:col + 1], sq[:, i * 128:(i + 1) * 128], T["onesb"][:, 0:1], start=False, stop=(c == 7), skip_group_check=True)
            PE.op(mm, ev=f"st{br}{G}_{c}")

        PE.wait("ones_set")
        PE.e.matmul(T["pst"][:, 0:8], T["onesb"][:, :], T["zerob"][:, 0:8], start=True, stop=False, skip_group_check=True)
        vbanks = ["pE", "pF"]
        n_v = 0
        for vj in range(4):
            PE.wait(wld("v", vj))
            W = wbuf(T, "v", vj)
            for i in range(4):
                bank = vbanks[n_v % 2]
                if n_v >= 2:
                    PE.wait(f"vg{G}_{n_v - 2}")
                for k in range(16):
                    mm = PE.e.matmul(T[bank][:, 0:256], hT[:, k, i * 128:(i + 1) * 128], W[:, k, :], start=(k == 0), stop=(k == 15))
                PE.op(mm, ev=f"pj_v{G}_{n_v}")
                n_v += 1
            S["pe"]._rec(f"w_free{G}_{bidx[('v', vj)]}", S["pe"].semname, cx.sy.counts[S["pe"].semname])
        for c in range(8):
            cj, cc = c // 2, c % 2
            bk = cbanks[c % 2]
            for n, bank in (("cg", bk[0]), ("xh", bk[1]), ("bg", bk[2])):
                PE.wait(wld(n, cj))
                if c >= 2:
                    PE.wait(f"ev_{n}{G}_{c - 2}")
                elif c == 1:
                    PE.wait(f"vg{G}_14", f"vg{G}_15")
                W = wbuf(T, n, cj)
                for k in range(16):
                    mm = PE.e.matmul(T[bank][:, :], W[:, k, cc * 128:(cc + 1) * 128], hT[:, k, :], start=(k == 0), stop=(k == 15))
                    if g == 0 and n in ("cg", "xh"):
                        hi = 0 if n == "cg" else 1
                        if k == 0 and c >= 1:
                            PE.wait(f"zh{G}_{c - 1}")
                        mm = PE.e.matmul(T["ph"][:, hi, :], W[:, k, cc * 128:(cc + 1) * 128], T["hTh"][:, k, :], start=(k == 0), stop=(k == 15), skip_group_check=True)
                PE.op(mm, ev=f"pj_{n}{G}_{c}")
                if cc == 1:
                    S["pe"]._rec(f"w_free{G}_{bidx[(n, cj)]}", S["pe"].semname, cx.sy.counts[S["pe"].semname])
            if c >= 1:
                stats(c - 1, "a")
        stats(7, "a")
        ubanks = ["pA", "pB", "pC", "pD"]
        for c in range(8):
            uj, cc = c // 2, c % 2
            PE.wait(wld("u", uj))
            bank = ubanks[c % 4]
            if c >= 4:
                PE.wait(f"ug{G}_{c - 4}")
            else:
                PE.wait(f"ev_cg{G}_{7}", f"ev_xh{G}_{7}", f"ev_bg{G}_{7}", f"ev_cg{G}_{6}", f"ev_xh{G}_{6}", f"ev_bg{G}_{6}")
            W = wbuf(T, "u", uj)
            for k in range(16):
                mm = PE.e.matmul(T[bank][:, :], W[:, k, cc * 128:(cc + 1) * 128], hT[:, k, :], start=(k == 0), stop=(k == 15))
            PE.op(mm, ev=f"pj_u{G}_{c}")
            if cc == 1:
                S["pe"]._rec(f"w_free{G}_{bidx[('u', uj)]}", S["pe"].semname, cx.sy.counts[S["pe"].semname])
        sbanks = ["pA", "pB"]
        for h in range(8):
            bank = sbanks[h % 2]
            if h >= 2:
                PE.wait(f"spt{G}_{h - 2}")
            else:
                PE.wait(f"ug{G}_{7}", f"ug{G}_{6}", f"ug{G}_{5}", f"ug{G}_{4}")
            for i in range(4):
                PE.wait(f"vnb{G}_{i}")
                mm = PE.e.matmul(T[bank][:, i * 128:(i + 1) * 128], T[f"vnb{i}"][:, h * 128:(h + 1) * 128], T["wsT"][:, h, :], start=True, stop=True)
            PE.op(mm, ev=f"pj_s{G}_{h}")
            if h >= 1:
                stats(h - 1, "b")
        stats(7, "b")

    def act(T):
        vbanks = ["pE", "pF"]
        n_v = 0
        for vj in range(4):
            for i in range(4):
                ACT.wait(f"pj_v{G}_{n_v}")
                ACT.op(ACT.e.activation(out=T[f"vg{i}"][:, vj * 256:(vj + 1) * 256], in_=T[vbanks[n_v % 2]][:, 0:256], func=AF.Gelu), ev=f"vg{G}_{n_v}")
                n_v += 1
        ACT.dep()
        for i in range(4):
            ACT.op(ACT.e.activation(out=T[f"vsq{i}"][:], in_=T[f"vg{i}"][:], func=AF.Square), ev=f"vsq{G}_{i}")
        ACT.wait(f"vvar{G}")
        ACT.op(ACT.e.activation(out=T["vs"][:, :, 8:16], in_=T["vs"][:, :, 8:16], func=AF.Sqrt), ev=f"vsqrt{G}")
        for c in range(8):
            bk = cbanks[c % 2]
            ACT.wait(f"pj_cg{G}_{c}")
            if c >= 1:
                ACT.wait(f"z{G}_{c - 1}")
            ACT.op(ACT.e.copy(out=T["t1"][:], in_=T[bk[0]][:, :]), ev=f"ev_cg{G}_{c}")
            if c >= 1:
                sqmix(T, c - 1, "a")
        sqmix(T, 7, "a")
        ubanks = ["pA", "pB", "pC", "pD"]
        for c in range(8):
            ACT.wait(f"pj_u{G}_{c}")
            ACT.op(ACT.e.activation(out=T["ug"][:, c, :], in_=T[ubanks[c % 4]][:, :], func=AF.Gelu), ev=f"ug{G}_{c}")
        for h in range(8):
            sqmix(T, h, "b")
        ACT.wait(f"stv{G}")
        ACT.op(ACT.e.activation(out=T["stt"][:], in_=T["stt"][:], func=AF.Sqrt), ev=f"stsq{G}")

    def sqmix(T, c, br):
        ACT.wait(f"raw{br}{G}_{c}")
        if c >= 2:
            ACT.wait(f"st{br}{G}_{c - 2}")
        elif br == "b":
            ACT.wait(f"sta{G}_{6 + c}")
        raw = T[f"raw{c % 2}"]
        ACT.op(ACT.e.activation(out=T[f"sq{c % 2}"][:], in_=raw[:], func=AF.Square), ev=f"sq{br}{G}_{c}")
        gn = T["goa"] if br == "a" else T["gob"]
        cm = c if br == "a" else 8 + c
        ACT.op(ACT.e.activation(out=T["mixin"][:, cm, :], in_=raw[:], func=AF.Copy, scale=gn[:, c:c + 1]), ev=f"mix{br}{G}_{c}")

    def dve(T):
        vs = T["vs"]
        for i in range(4):
            DVE.wait(f"vg{G}_{12 + i}")
            DVE.op(DVE.e.tensor_reduce(out=vs[:, i, 0:8], in_=T[f"vg{i}"][:].rearrange("p (h d) -> p h d", h=8), axis=AX.X, op=ALU.add))
            DVE.wait(f"vsq{G}_{i}")
            DVE.op(DVE.e.tensor_reduce(out=vs[:, i, 8:16], in_=T[f"vsq{i}"][:].rearrange("p (h d) -> p h d", h=8), axis=AX.X, op=ALU.add), ev=f"vs2{G}_{i}")
        DVE.dep()
        DVE.op(DVE.e.tensor_scalar(out=vs[:, :, 0:8], in0=vs[:, :, 0:8], scalar1=1.0 / 128, scalar2=None, op0=ALU.mult))
        DVE.dep()
        DVE.op(DVE.e.tensor_tensor(out=vs[:, :, 16:24], in0=vs[:, :, 0:8], in1=vs[:, :, 0:8], op=ALU.mult))
        DVE.op(DVE.e.tensor_scalar(out=vs[:, :, 8:16], in0=vs[:, :, 8:16], scalar1=1.0 / 128, scalar2=EPS, op0=ALU.mult, op1=ALU.add))
        DVE.dep()
        DVE.op(DVE.e.tensor_tensor(out=vs[:, :, 8:16], in0=vs[:, :, 8:16], in1=vs[:, :, 16:24], op=ALU.subtract), ev=f"vvar{G}")
        DVE.wait(f"vsqrt{G}")
        DVE.op(DVE.e.reciprocal(out=vs[:, :, 8:16], in_=vs[:, :, 8:16]))
        DVE.dep()
        for i in range(4):
            v3 = T[f"vg{i}"][:].rearrange("p (h d) -> p h d", h=8)
            n3 = T["vn"][:].rearrange("p (h d) -> p h d", h=8)
            DVE.op(DVE.e.tensor_tensor(out=n3, in0=v3, in1=bcast3(vs[:, i, 0:8], 8, 128), op=ALU.subtract))
            DVE.dep()
            DVE.op(DVE.e.tensor_tensor(out=n3, in0=n3, in1=bcast3(vs[:, i, 8:16], 8, 128), op=ALU.mult))
            DVE.dep()
            DVE.op(DVE.e.tensor_tensor(out=T["vn"][:], in0=T["vn"][:], in1=T["lngb"][:], op=ALU.mult))
            DVE.dep()
            DVE.op(DVE.e.tensor_tensor(out=T[f"vnb{i}"][:], in0=T["vn"][:], in1=T["lnbb"][:], op=ALU.add), ev=f"vnb{G}_{i}")
        z = T["z"]
        cw = T["convw"]
        for c in range(8):
            bk = cbanks[c % 2]
            if g == 0:
                DVE.wait(f"pj_xh{G}_{c}")
                DVE.op(DVE.e.tensor_copy(out=T["zh"][:], in_=T["ph"][:, 0, :]))
                DVE.dep()
                DVE.op(DVE.e.tensor_tensor(out=T["zh"][:], in0=T["zh"][:], in1=T["ph"][:, 1, :], op=ALU.mult), ev=f"zh{G}_{c}")
                DVE.dep()
                DVE.op(DVE.e.tensor_tensor(out=T["zhalo"][:, c, :], in0=T["zh"][:], in1=T["hmask"][:], op=ALU.mult))
                DVE.dep()
            DVE.wait(f"ev_cg{G}_{c}", f"pj_xh{G}_{c}")
            DVE.op(DVE.e.tensor_tensor(out=z[:, 1:GRP + 1], in0=T["t1"][:], in1=T[bk[1]][:, :], op=ALU.mult), ev=f"z{G}_{c}")
            S["dve"]._rec(f"ev_xh{G}_{c}", S["dve"].semname, cx.sy.counts[S["dve"].semname])
            DVE.op(DVE.e.tensor_copy(out=z[:, 0:1], in_=T["zhalo"][:, c, 2 * g:2 * g + 1]))
            DVE.op(DVE.e.tensor_copy(out=z[:, GRP + 1:GRP + 2], in_=T["zhalo"][:, c, 2 * g + 1:2 * g + 2]))
            DVE.dep()
            DVE.op(DVE.e.tensor_scalar(out=T["c0"][:], in0=z[:, 0:GRP], scalar1=cw[:, 0, c:c + 1], scalar2=None, op0=ALU.mult))
            DVE.dep()
            DVE.op(DVE.e.scalar_tensor_tensor(out=T["c1"][:], in0=z[:, 1:GRP + 1], scalar=cw[:, 1, c:c + 1], in1=T["c0"][:], op0=ALU.mult, op1=ALU.add))
            DVE.dep()
            DVE.op(DVE.e.scalar_tensor_tensor(out=T["c0"][:], in0=z[:, 2:GRP + 2], scalar=cw[:, 2, c:c + 1], in1=T["c1"][:], op0=ALU.mult, op1=ALU.add))
            DVE.dep()
            DVE.wait(f"pj_bg{G}_{c}")
            if c >= 2:
                DVE.wait(f"mixa{G}_{c - 2}")
            DVE.op(DVE.e.tensor_tensor(out=T[f"raw{c % 2}"][:], in0=T[bk[2]][:, :], in1=T["c0"][:], op=ALU.mult), ev=f"rawa{G}_{c}")
            S["dve"]._rec(f"ev_bg{G}_{c}", S["dve"].semname, cx.sy.counts[S["dve"].semname])
        sbanks = ["pA", "pB"]
        for h in range(8):
            DVE.wait(f"pj_s{G}_{h}")
            bsv = T["bsb"][:, h * 128:(h + 1) * 128].unsqueeze(1).to_broadcast([128, 4, 128])
            DVE.op(DVE.e.tensor_tensor(out=T["spt"][:].rearrange("p (i q) -> p i q", i=4), in0=T[sbanks[h % 2]][:, :].rearrange("p (i q) -> p i q", i=4), in1=bsv, op=ALU.add), ev=f"spt{G}_{h}")
            DVE.dep()
            if h >= 2:
                DVE.wait(f"mixb{G}_{h - 2}")
            else:
                DVE.wait(f"mixa{G}_{6 + h}")
            DVE.op(DVE.e.tensor_tensor(out=T[f"raw{h % 2}"][:], in0=T["spt"][:], in1=T["ug"][:, h, :], op=ALU.mult), ev=f"rawb{G}_{h}")
        DVE.wait(f"stb{G}_7", f"sta{G}_7")
        DVE.op(DVE.e.tensor_scalar(out=T["stt"][:], in0=T["pst"][:, 0:8], scalar1=1.0 / 1024, scalar2=EPS, op0=ALU.mult, op1=ALU.add), ev=f"stv{G}")
        DVE.wait(f"stsq{G}")
        DVE.op(DVE.e.reciprocal(out=T["rstdab"][:], in_=T["stt"][:]), ev=f"rstdab{G}")

    cx.phase("p2", specs, {"pool": pool, "act": act, "dve": dve, "pe": pe})


def phaseQ(cx, g):
    S = cx.S
    dr = cx.dr
    SP, PL, ACT, DVE, PE = S["sp"], S["pool"], S["act"], S["dve"], S["pe"]
    G = f"{g}"
    s = g // 2
    specs = [("wo0", [128, 16, 512], BF16), ("wo1", [128, 16, 512], BF16)] + [(f"mix{i}", [128, D], F32) for i in range(4)] + [
        ("xt0", [128, D], F32), ("xt1", [128, D], F32), ("G1b", [128, D], F32), ("A2b", [128, D], F32), ("B2b", [128, D], F32),
        ("junk", [128, D], BF16), ("h2b0", [128, D], BF16), ("h2b1", [128, D], BF16), ("h2T", [128, 16, 128], F32),
        ("ss", [128, 16], F32),
        ("pa0", [128, 512], F32, "psum"), ("pb0", [128, 512], F32, "psum"), ("pa1", [128, 512], F32, "psum"), ("pb1", [128, 512], F32, "psum"),
        ("ptr0", [128, 4, 128], F32, "psum"), ("ptr1", [128, 4, 128], F32, "psum"), ("plg", [128, 512], F32, "psum"),
    ]

    def pool(T):
        for ob in range(4):
            if ob >= 2:
                PL.wait(f"wo_free{G}_{ob - 2}")
            PL.dma(f"wo{ob % 2}", T[f"wo{ob % 2}"][:], dr["w_out"][:, ob * 512:(ob + 1) * 512].rearrange("(k p) n -> p k n", p=128), ev=f"wo_ld{G}_{ob}")

    def sp(T):
        SP.dma("qb", T["G1b"][:], dr["modv"][1, s:s + 1, :].partition_broadcast(128))
        SP.dma("qb", T["A2b"][:], dr["modv"][2, s:s + 1, :].partition_broadcast(128))
        SP.dma("qb", T["B2b"][:], dr["modv"][5, s:s + 1, :].partition_broadcast(128), ev="qb_ld" + G)
        for i in range(4):
            r0 = g * GRP + i * 128
            if i >= 2:
                SP.wait(f"x1st{G}_{i - 2}", f"h2f{G}_{i - 2}")
            SP.dma(f"qx{i % 2}", T[f"xt{i % 2}"][:], dr["xs"][r0:r0 + 128, :], ev=f"qx_ld{G}_{i}")
            if i >= 1:
                st(T, i - 1)
        st(T, 3)
        SP.wait(f"x1st{G}_2", f"h2st{G}_2", f"x1st{G}_3", f"h2st{G}_3")

    def st(T, i):
        r0 = g * GRP + i * 128
        SP.wait(f"x1{G}_{i}")
        SP.dma(f"qs{i % 2}", dr["x1"][r0:r0 + 128, :], T[f"xt{i % 2}"][:], ev=f"x1st{G}_{i}")
        SP.wait(f"h2b{G}_{i}")
        SP.dma(f"qh{i % 2}", dr["h2"][r0:r0 + 128, :], T[f"h2b{i % 2}"][:], ev=f"h2st{G}_{i}")

    def pe(T):
        mixin = T["mixin"]
        n = 0
        for ob in range(4):
            PE.wait(f"wo_ld{G}_{ob}")
            W = T[f"wo{ob % 2}"]
            for i in range(4):
                pa, pb = T[f"pa{n % 2}"], T[f"pb{n % 2}"]
                if n >= 2:
                    PE.wait(f"mixev{G}_{n - 2}")
                for c in range(8):
                    mm = PE.e.matmul(pa[:, :], mixin[:, c, i * 128:(i + 1) * 128], W[:, c, :], start=(c == 0), stop=(c == 7))
                for c in range(8, 16):
                    mm = PE.e.matmul(pb[:, :], mixin[:, c, i * 128:(i + 1) * 128], W[:, c, :], start=(c == 8), stop=(c == 15))
                PE.op(mm, ev=f"mm{G}_{n}")
                n += 1
            S["pe"]._rec(f"wo_free{G}_{ob}", S["pe"].semname, cx.sy.counts[S["pe"].semname])
        for i in range(4):
            PE.wait(f"h2f{G}_{i}")
            for q in range(4):
                ptr = T[f"ptr{q % 2}"]
                if i * 4 + q >= 2:
                    PE.wait(f"h2Tev{G}_{i * 4 + q - 2}")
                for kk in range(4):
                    k = q * 4 + kk
                    mm = PE.e.transpose(out=ptr[:, kk, :], in_=T[f"mix{i}"][:, k * 128:(k + 1) * 128], identity=T["identf"][:])
                PE.op(mm, ev=f"h2Tpe{G}_{i * 4 + q}")
            PE.wait(f"h2Tev{G}_{i * 4 + 2}", f"h2Tev{G}_{i * 4 + 3}")
            if i >= 1:
                PE.wait(f"lgev{G}_{i - 1}")
            for k in range(16):
                mm = PE.e.matmul(T["plg"][:, 0:NE], T["h2T"][:, k, :], T["wr"][:, k, :], start=(k == 0), stop=(k == 15))
            PE.op(mm, ev=f"lgpe{G}_{i}")

    def act(T):
        for i in range(4):
            ACT.wait(f"mixev{G}_{12 + i}")
            ACT.op(ACT.e.activation(out=T["junk"][:], in_=T[f"mix{i}"][:], func=AF.Square, accum_out=T["ss"][:, i:i + 1]), ev=f"qss{G}_{i}")
        ACT.wait(f"qvar{G}")
        ACT.op(ACT.e.activation(out=T["ss"][:, 4:8], in_=T["ss"][:, 4:8], func=AF.Sqrt), ev=f"qsqrt{G}")
        for i in range(4):
            ACT.wait(f"x1{G}_{i}")
            ACT.op(ACT.e.activation(out=T["junk"][:], in_=T[f"xt{i % 2}"][:], func=AF.Square, accum_out=T["ss"][:, 8 + i:9 + i]), ev=f"qss2{G}_{i}")
            ACT.wait(f"qvar2{G}_{i}")
            ACT.op(ACT.e.activation(out=T["ss"][:, 12 + i:13 + i], in_=T["ss"][:, 12 + i:13 + i], func=AF.Sqrt), ev=f"qsqrt2{G}_{i}")

    def dve(T):
        ra, rb = T["rstdab"][:, 0:4], T["rstdab"][:, 4:8]
        n = 0
        for ob in range(4):
            for i in range(4):
                DVE.wait(f"mm{G}_{n}")
                msl = T[f"mix{i}"][:, ob * 512:(ob + 1) * 512]
                DVE.op(DVE.e.tensor_scalar(out=msl, in0=T[f"pa{n % 2}"][:, :], scalar1=ra[:, i:i + 1], scalar2=None, op0=ALU.mult))
                DVE.dep()
                DVE.op(DVE.e.scalar_tensor_tensor(out=msl, in0=T[f"pb{n % 2}"][:, :], scalar=rb[:, i:i + 1], in1=msl, op0=ALU.mult, op1=ALU.add), ev=f"mixev{G}_{n}")
                n += 1
        ss = T["ss"]
        DVE.wait(f"qss{G}_3")
        DVE.op(DVE.e.tensor_scalar(out=ss[:, 4:8], in0=ss[:, 0:4], scalar1=1.0 / D, scalar2=EPS, op0=ALU.mult, op1=ALU.add), ev=f"qvar{G}")
        DVE.wait(f"qsqrt{G}")
        DVE.op(DVE.e.reciprocal(out=ss[:, 4:8], in_=ss[:, 4:8]))
        DVE.dep()
        DVE.wait("qb_ld" + G)

        def X1(i):
            xt = T[f"xt{i % 2}"]
            mix = T[f"mix{i}"]
            DVE.wait(f"qx_ld{G}_{i}")
            DVE.op(DVE.e.scalar_tensor_tensor(out=mix[:], in0=mix[:], scalar=ss[:, 4 + i:5 + i], in1=T["G1b"][:], op0=ALU.mult, op1=ALU.mult))
            DVE.dep()
            DVE.op(DVE.e.tensor_tensor(out=xt[:], in0=xt[:], in1=mix[:], op=ALU.add), ev=f"x1{G}_{i}")

        def H2(i):
            xt = T[f"xt{i % 2}"]
            mix = T[f"mix{i}"]
            DVE.wait(f"qss2{G}_{i}")
            DVE.op(DVE.e.tensor_scalar(out=ss[:, 12 + i:13 + i], in0=ss[:, 8 + i:9 + i], scalar1=1.0 / D, scalar2=EPS, op0=ALU.mult, op1=ALU.add), ev=f"qvar2{G}_{i}")
            DVE.wait(f"qsqrt2{G}_{i}")
            DVE.op(DVE.e.reciprocal(out=ss[:, 12 + i:13 + i], in_=ss[:, 12 + i:13 + i]))
            DVE.dep()
            DVE.op(DVE.e.scalar_tensor_tensor(out=mix[:], in0=xt[:], scalar=ss[:, 12 + i:13 + i], in1=T["A2b"][:], op0=ALU.mult, op1=ALU.mult))
            DVE.dep()
            DVE.op(DVE.e.tensor_tensor(out=mix[:], in0=mix[:], in1=T["B2b"][:], op=ALU.add), ev=f"h2f{G}_{i}")
            DVE.dep()
            if i >= 2:
                DVE.wait(f"h2st{G}_{i - 2}")
            DVE.op(DVE.e.tensor_copy(out=T[f"h2b{i % 2}"][:], in_=mix[:]), ev=f"h2b{G}_{i}")

        def RT(i):
            for qq in range(4):
                DVE.wait(f"h2Tpe{G}_{i * 4 + qq}")
                DVE.op(DVE.e.tensor_copy(out=T["h2T"][:, qq * 4:qq * 4 + 4, :], in_=T[f"ptr{qq % 2}"][:, :, :]), ev=f"h2Tev{G}_{i * 4 + qq}")
            DVE.wait(f"lgpe{G}_{i}")
            DVE.op(DVE.e.tensor_copy(out=T["LG"][:, g * 4 + i, :], in_=T["plg"][:, 0:NE]), ev=f"lgev{G}_{i}")

        X1(0)
        X1(1)
        H2(0)
        X1(2)
        RT(0)
        H2(1)
        X1(3)
        RT(1)
        H2(2)
        RT(2)
        H2(3)
        RT(3)

    cx.phase("q", specs, {"pool": pool, "sp": sp, "act": act, "dve": dve, "pe": pe})


def phaseR(cx):
    S = cx.S
    dr = cx.dr
    SP, PL, ACT, DVE, PE = S["sp"], S["pool"], S["act"], S["dve"], S["pe"]
    NL = NT * NE
    NGp = NT * 8
    big = ["SC", "SEL", "TMP", "EQ", "MSK", "M", "WF", "DD"]
    specs = [(n, [128, NL], F32) for n in big] + [
        ("Mb", [128, NL], BF16), ("g1", [128, NGp], F32), ("g2", [128, NGp], F32), ("gs", [128, NGp], F32), ("gm", [128, NGp], F32),
        ("geq", [128, NGp], F32), ("mx", [128, NT], F32), ("m8", [128, NT, 8], F32), ("i8", [128, NT, 8], U32),
        ("idc", [128, NGp], F32), ("sums", [128, NT], F32), ("destf", [128, NGp], F32), ("dA", [128, NGp], F32), ("dB", [128, NGp], F32),
        ("isB", [128, NGp], F32),
        ("pp0", [128, 512], F32, "psum"), ("pp1", [128, 512], F32, "psum"), ("pc", [128, 512], F32, "psum"),
    ]

    def v3(ap):
        return ap[:].rearrange("p (i e) -> p i e", e=NE)

    def v4(ap):
        return ap[:].rearrange("p (a j) -> p a j", j=8)

    def act(T):
        ACT.op(ACT.e.activation(out=T["SC"][:], in_=T["LG"][:].rearrange("p i e -> p (i e)"), func=AF.Sigmoid), ev="r_sc")

    def pe(T):
        PE.wait("r_Mb")
        for i in range(NT):
            pp = T[f"pp{i % 2}"]
            if i >= 2:
                PE.wait(f"r_dd{i - 2}")
            mm = PE.e.matmul(pp[:, 0:NE], T["trib"][:, :], T["Mb"][:, i * NE:(i + 1) * NE], start=True, stop=(i == 0))
            for j in range(i):
                mm = PE.e.matmul(pp[:, 0:NE], T["onesb"][:, :], T["Mb"][:, j * NE:(j + 1) * NE], start=False, stop=(j == i - 1))
            PE.op(mm, ev=f"r_pos{i}")
        for i in range(NT):
            mm = PE.e.matmul(T["pc"][:, 0:NE], T["onesb"][:, :], T["Mb"][:, i * NE:(i + 1) * NE], start=(i == 0), stop=(i == NT - 1))
        PE.op(mm, ev="r_cnt")

    def dve(T):
        def o(instr, ev=None):
            DVE.op(instr, ev=ev)
            DVE.dep()
        e = DVE.e
        SC, SEL, TMP, EQ, MSK, M, WF, DD = (T[n] for n in big)
        DVE.wait("r_sc")
        rb = T["rbias"][:, :].unsqueeze(1).to_broadcast([128, NT, NE])
        o(e.tensor_tensor(out=v3(SEL), in0=v3(SC), in1=rb, op=ALU.add))
        o(e.tensor_reduce(out=T["g1"][:], in_=v4(SEL), axis=AX.X, op=ALU.max))
        o(e.tensor_tensor(out=v4(EQ), in0=v4(SEL), in1=bcast3(T["g1"][:, :], NGp, 8), op=ALU.is_equal))
        o(e.scalar_tensor_tensor(out=TMP[:], in0=EQ[:], scalar=-BIG, in1=SEL[:], op0=ALU.mult, op1=ALU.add))
        o(e.tensor_reduce(out=T["g2"][:], in_=v4(TMP), axis=AX.X, op=ALU.max))
        o(e.tensor_tensor(out=T["gs"][:], in0=T["g1"][:], in1=T["g2"][:], op=ALU.add))
        o(e.memset(T["gm"][:], 0.0))
        for r in range(4):
            o(e.tensor_reduce(out=T["mx"][:], in_=v4(T["gs"]), axis=AX.X, op=ALU.max))
            o(e.tensor_tensor(out=v4(T["geq"]), in0=v4(T["gs"]), in1=bcast3(T["mx"][:, :], NT, 8), op=ALU.is_equal))
            o(e.tensor_tensor(out=T["gm"][:], in0=T["gm"][:], in1=T["geq"][:], op=ALU.add))
            o(e.scalar_tensor_tensor(out=T["gs"][:], in0=T["geq"][:], scalar=-BIG, in1=T["gs"][:], op0=ALU.mult, op1=ALU.add))
        o(e.tensor_tensor(out=v4(TMP), in0=v4(SEL), in1=bcast3(T["gm"][:, :], NGp, 8), op=ALU.mult))
        o(e.tensor_scalar(out=T["geq"][:], in0=T["gm"][:], scalar1=BIG, scalar2=-BIG, op0=ALU.mult, op1=ALU.add))
        o(e.tensor_tensor(out=v4(MSK), in0=v4(TMP), in1=bcast3(T["geq"][:, :], NGp, 8), op=ALU.add))
        for i in range(NT):
            DVE.op(e.max(out=T["m8"][:, i, :], in_=MSK[:, i * NE:(i + 1) * NE]))
        DVE.dep()
        for i in range(NT):
            DVE.op(e.max_index(out=T["i8"][:, i, :], in_max=T["m8"][:, i, :], in_values=MSK[:, i * NE:(i + 1) * NE]))
        DVE.dep()
        o(e.tensor_tensor(out=v3(M), in0=v3(MSK), in1=bcast3(T["m8"][:, :, 7], NT, NE), op=ALU.is_ge))
        o(e.tensor_copy(out=T["Mb"][:], in_=M[:]), ev="r_Mb")
        o(e.tensor_tensor(out=WF[:], in0=M[:], in1=SC[:], op=ALU.mult))
        o(e.tensor_reduce(out=T["sums"][:], in_=v3(WF), axis=AX.X, op=ALU.add))
        o(e.tensor_scalar(out=T["sums"][:], in0=T["sums"][:], scalar1=1e-20, scalar2=None, op0=ALU.add))
        o(e.reciprocal(out=T["sums"][:], in_=T["sums"][:]))
        o(e.scalar_tensor_tensor(out=v3(WF), in0=v3(WF), scalar=2.5, in1=bcast3(T["sums"][:, :], NT, NE), op0=ALU.mult, op1=ALU.mult))
        ec = T["ecap"][:, :]
        for i in range(NT):
            DVE.wait(f"r_pos{i}")
            DVE.op(e.tensor_tensor(out=DD[:, i * NE:(i + 1) * NE], in0=T[f"pp{i % 2}"][:, 0:NE], in1=ec, op=ALU.add), ev=f"r_dd{i}")
        DVE.dep()
        o(e.tensor_copy(out=T["idc"][:], in_=T["i8"][:].rearrange("p i k -> p (i k)")))
        o(e.tensor_scalar(out=T["idc"][:], in0=T["idc"][:], scalar1=float(CAP), scalar2=None, op0=ALU.mult))
        ecb = T["ecap"][:, :].unsqueeze(1).to_broadcast([128, NT, NE])
        idc3 = T["idc"][:].rearrange("p (i k) -> p i k", k=8)
        df3 = T["destf"][:].rearrange("p (i k) -> p i k", k=8)
        gk3 = T["gatek"][:].rearrange("p (i k) -> p i k", k=8)
        for k in range(8):
            o(e.tensor_tensor(out=v3(EQ), in0=ecb, in1=bcast3(idc3[:, :, k], NT, NE), op=ALU.is_equal))
            o(e.tensor_tensor(out=TMP[:], in0=EQ[:], in1=DD[:], op=ALU.mult))
            o(e.tensor_reduce(out=df3[:, :, k], in_=v3(TMP), axis=AX.X, op=ALU.add))
            o(e.tensor_tensor(out=TMP[:], in0=EQ[:], in1=WF[:], op=ALU.mult))
            o(e.tensor_reduce(out=gk3[:, :, k], in_=v3(TMP), axis=AX.X, op=ALU.add))
        half = float(NE // 2 * CAP)
        o(e.tensor_copy(out=T["desti"][:], in_=T["destf"][:]))
        o(e.tensor_scalar(out=T["isB"][:], in0=T["destf"][:], scalar1=half, scalar2=None, op0=ALU.is_ge))
        HUGE = 4.0e6
        o(e.scalar_tensor_tensor(out=T["dA"][:], in0=T["isB"][:], scalar=HUGE, in1=T["destf"][:], op0=ALU.mult, op1=ALU.add))
        o(e.tensor_scalar(out=T["dB"][:], in0=T["isB"][:], scalar1=-HUGE, scalar2=HUGE - half, op0=ALU.mult, op1=ALU.add))
        o(e.tensor_tensor(out=T["dB"][:], in0=T["dB"][:], in1=T["destf"][:], op=ALU.add))
        o(e.tensor_copy(out=T["destA"][:], in_=T["dA"][:]))
        o(e.tensor_copy(out=T["destB"][:], in_=T["dB"][:]))
        DVE.wait("r_cnt")
        o(e.tensor_copy(out=T["cnti"][:], in_=T["pc"][:, 0:NE]), ev="r_done")

    cx.phase("r", specs, {"act": act, "dve": dve, "pe": pe})


def phaseD(cx):
    S = cx.S
    dr = cx.dr
    SP, PL = S["sp"], S["pool"]
    specs = [(f"hb{i}", [128, D], BF16) for i in range(3)]

    def sp(T):
        for i in range(NT):
            if i >= 3:
                SP.wait(f"d_sc{i - 3}")
            SP.dma(f"dh{i % 3}", T[f"hb{i % 3}"][:], dr["h2"][i * 128:(i + 1) * 128, :], ev=f"d_ld{i}")

    def pool(T):
        bc = PL.e.alloc_register("bcD")
        PL.e.reg_mov(bc, NE * CAP - 1)
        for i in range(NT):
            PL.wait(f"d_ld{i}")
            for k in range(8):
                c = i * 8 + k
                PL.idma(f"ds{i % 3}", out=dr["xsl"], out_idx=T["desti"][:, c:c + 1],
                        in_=T[f"hb{i % 3}"][:, :], bounds_check=bc, oob_is_err=False,
                        ev=(f"d_sc{i}" if k == 7 else None))
        for i in range(NT - 3, NT):
            PL.wait(f"d_sc{i}")

    cx.phase("d", specs, {"sp": sp, "pool": pool})


def phaseM(cx, experts=None):
    S = cx.S
    dr = cx.dr
    SP, PL, ACT, DVE, PE = S["sp"], S["pool"], S["act"], S["dve"], S["pe"]
    NWB = 3
    NWD = 2
    XR = 4
    YR = 3
    specs = [(f"wg{i}", [128, 16, FF], BF16) for i in range(NWB)] + [(f"wu{i}", [128, 16, FF], BF16) for i in range(NWB)] + \
            [(f"wd{i}", [128, 4, D], BF16) for i in range(NWD)] + \
            [(f"xin{i}", [128, D], BF16) for i in range(XR)] + [(f"xT{i}", [128, 16, 128], BF16) for i in range(2)] + \
            [(f"sg{i}", [128, FF], F32) for i in range(2)] + [(f"ac{i}", [128, FF], BF16) for i in range(2)] + \
            [(f"aT{i}", [128, 4, 128], BF16) for i in range(2)] + [(f"yb{i}", [128, D], F32) for i in range(YR)] + [("scr", [128, 8], BF16)] + [
        ("TX", [128, 16, 128], BF16, "psum"), ("PG", [128, 512], F32, "psum"), ("PU", [128, 512], F32, "psum"),
        ("Y0", [128, 512], F32, "psum"), ("Y1", [128, 512], F32, "psum"), ("Y2", [128, 512], F32, "psum"), ("Y3", [128, 512], F32, "psum"),
    ]
    if experts is None:
        experts = list(range(NE + 1))
    blocks = []
    for ei, e in enumerate(experts):
        nb = JBLK if e < NE else NT
        for j in range(nb):
            if e < NE:
                src = dr["xsl"][e * CAP + j * 128:e * CAP + (j + 1) * 128, :]
                if e < NE // 2:
                    dst = dr["ysl0"][e * CAP + j * 128:e * CAP + (j + 1) * 128, :]
                else:
                    e2 = e - NE // 2
                    dst = dr["ysl1"][e2 * CAP + j * 128:e2 * CAP + (j + 1) * 128, :]
            else:
                src = dr["h2"][j * 128:(j + 1) * 128, :]
                dst = dr["ysh"][j * 128:(j + 1) * 128, :]
            blocks.append((ei, e, j, src, dst, j == nb - 1))
    NB = len(blocks)
    eblocks = {}
    for b, blk in enumerate(blocks):
        eblocks.setdefault(blk[0], []).append(b)

    def xidx(b):
        return (blocks[b][0] % 2) * 2 + (blocks[b][2] % 2)

    def wsrc(e):
        if e < NE:
            return dr["w_exp_gate"][e], dr["w_exp_up"][e], dr["w_exp_down"][e]
        return dr["w_sh_gate"], dr["w_sh_up"], dr["w_sh_down"]

    def pool(T):
        for ei, e in enumerate(experts):
            wgs, wus, wds = wsrc(e)
            if ei >= NWB:
                PL.wait(f"m_gulast{ei - NWB}")
            PL.dma(f"mw{ei % NWB}", T[f"wg{ei % NWB}"][:], wgs.rearrange("(k p) n -> p k n", p=128))
            PL.dma(f"mw{ei % NWB}", T[f"wu{ei % NWB}"][:], wus.rearrange("(k p) n -> p k n", p=128))
            S["pool"]._rec(f"m_wld{ei}", "d_" + f"mw{ei % NWB}", cx.sy.counts["d_" + f"mw{ei % NWB}"])
            if ei >= NWD:
                PL.wait(f"m_dlast{ei - NWD}")
            PL.dma(f"md{ei % NWD}", T[f"wd{ei % NWD}"][:], wds.rearrange("(c p) n -> p c n", p=128), ev=f"m_wdld{ei}")

    def make_cond(St, T):
        state = {"regs": None, "loaded": set()}

        def cond(b):
            ei, e, j = blocks[b][0], blocks[b][1], blocks[b][2]
            if e >= NE or cx.sy.dry or not DYN_SKIP:
                return None
            if state["regs"] is None:
                state["regs"] = [St.e.alloc_register(f"cnt{St.name}{p}") for p in range(2)]
            if ei not in state["loaded"]:
                state["loaded"].add(ei)
                St.e.reg_load(state["regs"][ei % 2], T["cnti"][0:1, e:e + 1])
            return (state["regs"][ei % 2], j * 128)
        return cond

    def sp(T):
        cond = make_cond(SP, T)
        sy = cx.sy

        def dma_piece(b, waits, key, out, in_, ev):
            c = cond(b)
            if sy.dry or c is None:
                SP.wait(*waits)
                SP.dma(key, out, in_, ev=ev)
                return
            w0 = dict(SP.waited)
            with SP.e.If_cmp(c[0], c[1], "IS_GT"):
                SP.wait(*waits)
                SP.dma(key, out, in_, ev=ev)
            w1 = dict(SP.waited)
            SP.waited = w0
            with SP.e.Else():
                SP.wait(*waits)
                SP.e.sem_inc(sy.sems["d_" + key], 16)
            SP.waited = w1

        def store(b):
            dma_piece(b, [f"m_yev{b}"], f"my{b % YR}", blocks[b][4], T[f"yb{b % YR}"][:], f"m_yst{b}")

        def load(b):
            ei, j = blocks[b][0], blocks[b][2]
            xi = xidx(b)
            if j >= 2:
                waits = [f"m_tx{b - 2}"]
            elif ei >= 2:
                prev = [bb for bb in eblocks[ei - 2] if blocks[bb][2] % 2 == j % 2]
                waits = [f"m_tx{prev[-1]}"]
            else:
                waits = []
            dma_piece(b, waits, f"mx{xi}", T[f"xin{xi}"][:], blocks[b][3], f"m_xld{b}")

        ne = len(experts)
        for b in eblocks[0][:2]:
            load(b)
        for ei in range(ne):
            if ei + 1 < ne:
                for b in eblocks[ei + 1][:2]:
                    load(b)
            bl = eblocks[ei]
            for j, b in enumerate(bl):
                if j + 2 < len(bl):
                    load(bl[j + 2])
                store(b)
        for b in range(max(0, NB - YR), NB):
            SP.wait(f"m_yst{b}")


    def expert_reg(St, T, regs, ei):
        e = experts[ei]
        if e >= NE or cx.sy.dry or not DYN_SKIP:
            return None
        if not regs:
            regs.extend([St.e.alloc_register(f"cn{St.name}{p}") for p in range(2)])
        St.e.reg_load(regs[ei % 2], T["cnti"][0:1, e:e + 1])
        return regs[ei % 2]

    def pe(T):
        regs = []

        def Tx(b):
            def body():
                PE.wait(f"m_xld{b}", *([f"m_xTev{b - 1}"] if b >= 1 else []))
                for k in range(16):
                    mm = PE.e.transpose(out=T["TX"][:, k, :], in_=T[f"xin{xidx(b)}"][:, k * 128:(k + 1) * 128], identity=T["identb"][:])
                PE.op(mm, ev=f"m_tx{b}")
            return body

        def GU(b):
            ei = blocks[b][0]

            def body():
                PE.wait(f"m_wld{ei}", f"m_xTev{b}", *([f"m_act{b - 1}", f"m_aTev{b - 1}", f"m_sg{b - 1}"] if b >= 1 else []))
                xT = T[f"xT{b % 2}"]
                wg, wu = T[f"wg{ei % NWB}"], T[f"wu{ei % NWB}"]
                for k in range(16):
                    PE.e.matmul(T["PG"][:, :], xT[:, k, :], wg[:, k, :], start=(k == 0), stop=(k == 15))
                    mm = PE.e.matmul(T["PU"][:, :], xT[:, k, :], wu[:, k, :], start=(k == 0), stop=(k == 15))
                PE.op(mm, ev=f"m_gu{b}")
                if blocks[b][5]:
                    S["pe"]._rec(f"m_gulast{ei}", S["pe"].semname, cx.sy.counts[S["pe"].semname])
            return body

        def TA(b):
            def body():
                PE.wait(f"m_act{b}")
                ac = T[f"ac{b % 2}"]
                pga = T["PG"][:, :].bitcast(BF16).rearrange("p (c t) -> p c t", t=128)
                for c in range(4):
                    mm = PE.e.transpose(out=pga[:, c, :], in_=ac[:, c * 128:(c + 1) * 128], identity=T["identb"][:])
                PE.op(mm, ev=f"m_ta{b}")
            return body

        def Dn(b):
            ei = blocks[b][0]

            def body():
                PE.wait(f"m_wdld{ei}", f"m_aTev{b}", *([f"m_yev{b - 1}"] if b >= 1 else []))
                aT = T[f"aT{b % 2}"]
                wd = T[f"wd{ei % NWD}"]
                for nb in range(4):
                    for c in range(4):
                        mm = PE.e.matmul(T[f"Y{nb}"][:, :], aT[:, c, :], wd[:, c, nb * 512:(nb + 1) * 512], start=(c == 0), stop=(c == 3))
                PE.op(mm, ev=f"m_d{b}")
                if blocks[b][5]:
                    S["pe"]._rec(f"m_dlast{ei}", S["pe"].semname, cx.sy.counts[S["pe"].semname])
            return body

        def tiny(k):
            PE.tick(PE.e.transpose(out=T["TX"][0:1, 0, :], in_=T["identb"][:, 0:1], identity=T["identb"][:]), k)

        PE.wait("ones_set")
        for ei in range(len(experts)):
            bl = eblocks[ei]
            pieces = [(0, 1, Tx(bl[0])), (0, 1, GU(bl[0]))]
            for j, b in enumerate(bl):
                if j + 1 < len(bl):
                    pieces.append((j + 1, 1, Tx(bl[j + 1])))
                pieces.append((j, 1, TA(b)))
                pieces.append((j, 1, Dn(b)))
                if j + 1 < len(bl):
                    pieces.append((j + 1, 1, GU(bl[j + 1])))
            PE.emit_expert(pieces, expert_reg(PE, T, regs, ei), tiny)

    def act(T):
        for b in range(NB):
            ACT.wait(f"m_gu{b}")
            if b >= 2:
                ACT.wait(f"m_act{b - 2}")
            ACT.op(ACT.e.activation(out=T[f"sg{b % 2}"][:], in_=T["PG"][:, :], func=AF.Silu), ev=f"m_sg{b}")

    def dve(T):
        regs = []

        def xTev(b):
            def body():
                DVE.wait(f"m_tx{b}", *([f"m_gu{b - 2}"] if b >= 2 else []))
                DVE.op(DVE.e.tensor_copy(out=T[f"xT{b % 2}"][:], in_=T["TX"][:]), ev=f"m_xTev{b}")
            return body

        def act_(b):
            def body():
                DVE.wait(f"m_sg{b}", *([f"m_ta{b - 2}"] if b >= 2 else []))
                DVE.op(DVE.e.tensor_tensor(out=T[f"ac{b % 2}"][:], in0=T[f"sg{b % 2}"][:], in1=T["PU"][:, :], op=ALU.mult), ev=f"m_act{b}")
            return body

        def aTev(b):
            def body():
                DVE.wait(f"m_ta{b}", *([f"m_d{b - 2}"] if b >= 2 else []))
                pga = T["PG"][:, :].bitcast(BF16).rearrange("p (c t) -> p c t", t=128)
                DVE.op(DVE.e.tensor_copy(out=T[f"aT{b % 2}"][:], in_=pga[:, 0:4, :]), ev=f"m_aTev{b}")
            return body

        def yev(b):
            def body():
                DVE.wait(f"m_d{b}", *([f"m_yst{b - YR}"] if b >= YR else []))
                for nb in range(4):
                    DVE.op(DVE.e.tensor_copy(out=T[f"yb{b % YR}"][:, nb * 512:(nb + 1) * 512], in_=T[f"Y{nb}"][:, :]), ev=(f"m_yev{b}" if nb == 3 else None))
            return body

        def tiny(k):
            DVE.tick(DVE.e.tensor_copy(out=T["scr"][:, 0:1], in_=T["zerob"][:, 0:1]), k)

        for ei in range(len(experts)):
            bl = eblocks[ei]
            pieces = [(0, 1, xTev(bl[0]))]
            for j, b in enumerate(bl):
                pieces.append((j, 1, act_(b)))
                pieces.append((j, 1, aTev(b)))
                if j + 1 < len(bl):
                    pieces.append((j + 1, 1, xTev(bl[j + 1])))
                pieces.append((j, 4, yev(b)))
            DVE.emit_expert(pieces, expert_reg(DVE, T, regs, ei), tiny)

    cx.phase("m", specs, {"pool": pool, "sp": sp, "act": act, "dve": dve, "pe": pe})


def phaseF(cx):
    S = cx.S
    dr = cx.dr
    SP, PL, ACT, DVE, PE = S["sp"], S["pool"], S["act"], S["dve"], S["pe"]
    NGK = 4
    specs = [(f"gk{i}", [128, D], F32) for i in range(NGK)] + [(f"acc{i}", [128, D], F32) for i in range(2)] + \
            [(f"x1t{i}", [128, D], F32) for i in range(2)] + [("G2b0", [128, D], F32), ("G2b1", [128, D], F32), ("junk", [128, D], BF16), ("ss", [128, 2 * NT], F32)]
    half = NE // 2 * CAP

    def pool(T):
        bc = PL.e.alloc_register("bcF")
        PL.e.reg_mov(bc, half - 1)
        n = 0
        for i in range(NT):
            for k in range(8):
                c = i * 8 + k
                if n >= NGK:
                    PL.wait(f"f_fma{n - NGK}")
                gk = T[f"gk{n % NGK}"]
                PL.idma(f"fg{n % NGK}", out=gk[:, :], in_=dr["ysl0"],
                        in_idx=T["destA"][:, c:c + 1], bounds_check=bc, oob_is_err=False)
                PL.idma(f"fg{n % NGK}", out=gk[:, :], in_=dr["ysl1"],
                        in_idx=T["destB"][:, c:c + 1], bounds_check=bc, oob_is_err=False,
                        ev=f"f_g{n}")
                n += 1

    def sp(T):
        def store(i):
            SP.wait(f"f_y{i}")
            SP.dma(f"fy{i % 2}", dr["y"][i * 128:(i + 1) * 128, :], T[f"x1t{i % 2}"][:], ev=f"f_yst{i}")
        for i in range(NT):
            s = i // 8
            if i % 8 == 0:
                if s >= 2:
                    SP.wait(f"f_y{(s - 1) * 8 - 1}")
                SP.dma(f"fG{s % 2}", T[f"G2b{s % 2}"][:], dr["modv"][3, s:s + 1, :].partition_broadcast(128), ev=f"f_G{s}")
            if i >= 2:
                SP.wait(f"f_y{i - 2}")
            SP.dma(f"fa{i % 2}", T[f"acc{i % 2}"][:], dr["ysh"][i * 128:(i + 1) * 128, :], ev=f"f_acc{i}")
            if i >= 2:
                SP.wait(f"f_yst{i - 2}")
            SP.dma(f"fx{i % 2}", T[f"x1t{i % 2}"][:], dr["x1"][i * 128:(i + 1) * 128, :], ev=f"f_x1{i}")
            if i >= 1:
                store(i - 1)
        store(NT - 1)
        SP.wait(f"f_yst{NT - 1}", f"f_yst{NT - 2}")

    def act(T):
        for i in range(NT):
            ACT.wait(f"f_sum{i}")
            ACT.op(ACT.e.activation(out=T["junk"][:], in_=T[f"acc{i % 2}"][:], func=AF.Square, accum_out=T["ss"][:, i:i + 1]), ev=f"f_ss{i}")
            ACT.wait(f"f_var{i}")
            ACT.op(ACT.e.activation(out=T["ss"][:, NT + i:NT + i + 1], in_=T["ss"][:, NT + i:NT + i + 1], func=AF.Sqrt), ev=f"f_sq{i}")

    def dve(T):
        n = 0
        ss = T["ss"]
        for i in range(NT):
            acc = T[f"acc{i % 2}"]
            DVE.wait(f"f_acc{i}")
            for k in range(8):
                c = i * 8 + k
                DVE.wait(f"f_g{n}")
                DVE.op(DVE.e.scalar_tensor_tensor(out=acc[:], in0=T[f"gk{n % NGK}"][:], scalar=T["gatek"][:, c:c + 1], in1=acc[:], op0=ALU.mult, op1=ALU.add),
                       ev=f"f_fma{n}")
                DVE.dep()
                n += 1
            S["dve"]._rec(f"f_sum{i}", S["dve"].semname, cx.sy.counts[S["dve"].semname])
            DVE.wait(f"f_ss{i}")
            DVE.op(DVE.e.tensor_scalar(out=ss[:, NT + i:NT + i + 1], in0=ss[:, i:i + 1], scalar1=1.0 / D, scalar2=EPS, op0=ALU.mult, op1=ALU.add), ev=f"f_var{i}")
            DVE.wait(f"f_sq{i}")
            DVE.op(DVE.e.reciprocal(out=ss[:, NT + i:NT + i + 1], in_=ss[:, NT + i:NT + i + 1]))
            DVE.dep()
            DVE.wait(f"f_G{i // 8}", f"f_x1{i}")
            DVE.op(DVE.e.scalar_tensor_tensor(out=acc[:], in0=acc[:], scalar=ss[:, NT + i:NT + i + 1], in1=T[f"G2b{(i // 8) % 2}"][:], op0=ALU.mult, op1=ALU.mult))
            DVE.dep()
            DVE.op(DVE.e.tensor_tensor(out=T[f"x1t{i % 2}"][:], in0=T[f"x1t{i % 2}"][:], in1=acc[:], op=ALU.add), ev=f"f_y{i}")

    cx.phase("f", specs, {"pool": pool, "sp": sp, "act": act, "dve": dve})


_CACHE = {}


def kernel(**inputs):
    inputs = {k: np.asarray(v) for k, v in inputs.items()}
    if "nc" not in _CACHE:
        _CACHE["nc"] = build_program()
    nc = _CACHE["nc"]
    in_maps = [make_inputs_for_core(c, inputs) for c in range(NCORES)]
    res = run_bass_kernel_spmd(nc, in_maps, core_ids=list(range(NCORES)))
    ys = [np.asarray(r["y"]) for r in res.results]
    y_prompt = np.empty((2, 8192, D), np.float32)
    y_sample = np.empty((1, 8192, D), np.float32)
    for c in range(NCORES):
        lo = c * SEGT
        y_prompt[0, lo:lo + SEGT] = ys[c][0:SEGT]
        y_prompt[1, lo:lo + SEGT] = ys[c][SEGT:2 * SEGT]
        y_sample[0, lo:lo + SEGT] = ys[c][2 * SEGT:3 * SEGT]
    return (y_prompt, y_sample)
```
